# Optimizing a Trainium2 kernel written in Bass

```python
import math
import jax, jax.numpy as jnp
from jax import lax
import numpy as np

D_MODEL = 1024
BATCH = 8
SEQ = 4096
DEPTH = 1

EPS = 1e-6
SSD_HEAD_DIM = 64
D_SSD = D_MODEL
SSD_HEADS = D_SSD // SSD_HEAD_DIM
SSD_GROUPS = 2
SSD_STATE = 128
CONV_K = 4
SSD_CHUNK = 128
CONV_CH = D_SSD + 2 * SSD_GROUPS * SSD_STATE
ATTN_HEAD_DIM = 64
D_ATTN = D_MODEL
ATTN_Q_HEADS = D_ATTN // ATTN_HEAD_DIM
ATTN_KV_HEADS = 2
Q_PER_KV = ATTN_Q_HEADS // ATTN_KV_HEADS
D_KV = ATTN_KV_HEADS * ATTN_HEAD_DIM
WINDOW = 128
ATTN_SCALE = ATTN_HEAD_DIM ** -0.5
D_MIX = D_SSD + D_ATTN
SPLITS = (D_SSD,
          D_SSD + CONV_CH,
          D_SSD + CONV_CH + SSD_HEADS,
          D_SSD + CONV_CH + SSD_HEADS + D_ATTN,
          D_SSD + CONV_CH + SSD_HEADS + D_ATTN + D_KV)
IN_DIM = D_SSD + CONV_CH + SSD_HEADS + D_ATTN + 2 * D_KV
MOE_GROUPS = 8
EXPERTS_PER_GROUP = 8
N_EXPERTS = MOE_GROUPS * EXPERTS_PER_GROUP
TOP_K = 2
D_FF_EXPERT = D_MODEL // 2
MOE_BLOCK = 128

kernel_name = "hymba_ssd_swa_sink_hmoe_block"


def rmsnorm(x, g):
    xf = x.astype(jnp.float32)
    y = xf * lax.rsqrt(jnp.mean(xf * xf, axis=-1, keepdims=True) + EPS)
    return (y * g.astype(jnp.float32)).astype(x.dtype)


def causal_depthwise_conv(u, w, b):
    c = u.shape[-1]
    y = lax.conv_general_dilated(u, w[:, None, :], window_strides=(1,),
                                 padding=[(CONV_K - 1, 0)],
                                 dimension_numbers=("NWC", "WIO", "NWC"),
                                 feature_group_count=c)
    return y + b


def ssd_chunked(x, dt, A, B, C):
    b, s, h, p = x.shape
    g, n = B.shape[2], B.shape[3]
    r = h // g
    c = s // SSD_CHUNK
    L = SSD_CHUNK
    xc = x.reshape(b, c, L, g, r, p)
    dtc = dt.reshape(b, c, L, g, r)
    Bc = B.reshape(b, c, L, g, n)
    Cc = C.reshape(b, c, L, g, n)
    a = jnp.moveaxis(dtc * A.reshape(g, r), 2, -1)
    a_cs = jnp.cumsum(a, axis=-1)
    xdt = xc * dtc[..., None]
    idx = jnp.arange(L)
    causal = idx[:, None] >= idx[None, :]
    seg = a_cs[..., :, None] - a_cs[..., None, :]
    decay = jnp.exp(jnp.where(causal, seg, -jnp.inf))
    cb = jnp.einsum("bclgn,bcsgn->bcgls", Cc, Bc)
    y_diag = jnp.einsum("bcgls,bcgrls,bcsgrp->bclgrp", cb, decay, xdt)
    decay_to_end = jnp.exp(a_cs[..., -1:] - a_cs)
    states = jnp.einsum("bclgn,bcgrl,bclgrp->bcgrpn", Bc, decay_to_end, xdt)
    chunk_decay = jnp.exp(a_cs[..., -1])

    def step(carry, inp):
        st, dec = inp
        return carry * dec[..., None, None] + st, carry

    init = jnp.zeros((b, g, r, p, n), states.dtype)
    _, prev = lax.scan(step, init, (jnp.moveaxis(states, 1, 0), jnp.moveaxis(chunk_decay, 1, 0)))
    prev = jnp.moveaxis(prev, 0, 1)
    y_off = jnp.einsum("bclgn,bcgrpn,bcgrl->bclgrp", Cc, prev, jnp.exp(a_cs))
    return (y_diag + y_off).reshape(b, s, h, p)


def gated_group_rmsnorm(y, z, gain):
    u = y * jax.nn.silu(z.astype(jnp.float32))
    u = u.reshape(*y.shape[:-1], SSD_GROUPS, -1)
    u = u * lax.rsqrt(jnp.mean(u * u, axis=-1, keepdims=True) + EPS)
    return u.reshape(y.shape) * gain.astype(jnp.float32)


def swa_gqa_sinks(q, k, v, sinks):
    b, s = q.shape[0], q.shape[1]
    nb = s // WINDOW
    W = WINDOW
    qb = q.astype(jnp.float32).reshape(b, nb, W, ATTN_KV_HEADS, Q_PER_KV, ATTN_HEAD_DIM) * ATTN_SCALE
    kb = k.astype(jnp.float32).reshape(b, nb, W, ATTN_KV_HEADS, ATTN_HEAD_DIM)
    vb = v.astype(jnp.float32).reshape(b, nb, W, ATTN_KV_HEADS, ATTN_HEAD_DIM)
    pad = ((0, 0), (1, 0), (0, 0), (0, 0), (0, 0))
    kcat = jnp.concatenate([jnp.pad(kb, pad)[:, :-1], kb], axis=2)
    vcat = jnp.concatenate([jnp.pad(vb, pad)[:, :-1], vb], axis=2)
    scores = jnp.einsum("bnqhrd,bnkhd->bnhrqk", qb, kcat)
    qi = jnp.arange(W)[:, None]
    kj = jnp.arange(2 * W)[None, :]
    rel = qi + W - kj
    band = (rel >= 0) & (rel < WINDOW)
    blk = jnp.arange(nb)[:, None, None]
    valid = band[None] & ((blk > 0) | (kj >= W)[None])
    scores = jnp.where(valid[None, :, None, None], scores, -jnp.inf)
    sink = sinks.astype(jnp.float32).reshape(ATTN_KV_HEADS, Q_PER_KV)[None, None, :, :, None, None]
    m = jnp.maximum(jnp.max(scores, axis=-1, keepdims=True), sink)
    pr = jnp.exp(scores - m)
    denom = jnp.sum(pr, axis=-1, keepdims=True) + jnp.exp(sink - m)
    out = jnp.einsum("bnhrqk,bnkhd->bnqhrd", pr / denom, vcat)
    return out.reshape(b, s, ATTN_Q_HEADS * ATTN_HEAD_DIM)


def hybrid_mixer(h, w_in, conv_w, conv_b, dt_bias, a_log, d_skip, ssd_norm,
                 attn_sinks, attn_norm, w_out):
    b, s, _ = h.shape
    proj = h @ w_in
    z, xbc, dt_raw, q, k, v = jnp.split(proj, SPLITS, axis=-1)
    xbc = jax.nn.silu(causal_depthwise_conv(xbc, conv_w, conv_b)).astype(jnp.float32)
    xs, Bm, Cm = jnp.split(xbc, (D_SSD, D_SSD + SSD_GROUPS * SSD_STATE), axis=-1)
    xs = xs.reshape(b, s, SSD_HEADS, SSD_HEAD_DIM)
    dt = jax.nn.softplus(dt_raw.astype(jnp.float32) + dt_bias.astype(jnp.float32))
    A = -jnp.exp(a_log.astype(jnp.float32))
    y = ssd_chunked(xs, dt, A,
                    Bm.reshape(b, s, SSD_GROUPS, SSD_STATE),
                    Cm.reshape(b, s, SSD_GROUPS, SSD_STATE))
    y = y + xs * d_skip.astype(jnp.float32)[:, None]
    y_ssd = gated_group_rmsnorm(y.reshape(b, s, D_SSD), z, ssd_norm)
    att = swa_gqa_sinks(q.reshape(b, s, ATTN_Q_HEADS, ATTN_HEAD_DIM),
                        k.reshape(b, s, ATTN_KV_HEADS, ATTN_HEAD_DIM),
                        v.reshape(b, s, ATTN_KV_HEADS, ATTN_HEAD_DIM), attn_sinks)
    y_att = rmsnorm(att, attn_norm)
    mixed = jnp.concatenate([y_ssd, y_att], axis=-1).astype(h.dtype)
    return mixed @ w_out


def hierarchical_moe(h, w_router_group, w_router_expert, w_gate, w_up, w_down):
    b, s, d = h.shape
    t = h.reshape(-1, d)
    n = t.shape[0]
    rows = jnp.arange(n)
    group_logits = (t @ w_router_group).astype(jnp.float32)
    group_prob = jax.nn.softmax(group_logits, axis=-1)
    g_sel = jnp.argmax(group_logits, axis=-1)
    p_group = group_prob[rows, g_sel][:, None]
    exp_logits = (t @ w_router_expert).astype(jnp.float32).reshape(n, MOE_GROUPS, EXPERTS_PER_GROUP)
    in_group = exp_logits[rows, g_sel]
    top_logit, top_local = lax.top_k(in_group, TOP_K)
    gate = jax.nn.softmax(top_logit, axis=-1) * p_group
    expert_id = g_sel[:, None] * EXPERTS_PER_GROUP + top_local
    a = n * TOP_K
    flat_e = expert_id.reshape(-1)
    flat_g = gate.reshape(-1)
    order = jnp.argsort(flat_e)
    e_sorted = flat_e[order]
    tok_sorted = order // TOP_K
    counts = jnp.bincount(flat_e, length=N_EXPERTS)
    padded = ((counts + MOE_BLOCK - 1) // MOE_BLOCK) * MOE_BLOCK
    start = jnp.cumsum(counts) - counts
    pend = jnp.cumsum(padded)
    pstart = pend - padded
    dest = pstart[e_sorted] + jnp.arange(a) - start[e_sorted]
    n_blocks = -(-(a + N_EXPERTS * (MOE_BLOCK - 1)) // MOE_BLOCK)
    buf = jnp.zeros((n_blocks * MOE_BLOCK, d), t.dtype).at[dest].set(t[tok_sorted])
    block_expert = jnp.minimum(
        jnp.searchsorted(pend, jnp.arange(n_blocks) * MOE_BLOCK, side="right"), N_EXPERTS - 1)

    def expert_block(args):
        xb, e = args
        hid = jax.nn.silu(xb @ w_gate[e]) * (xb @ w_up[e])
        return hid @ w_down[e]

    y_buf = lax.map(expert_block, (buf.reshape(n_blocks, MOE_BLOCK, d), block_expert))
    y_buf = y_buf.reshape(-1, d)
    y = jnp.zeros_like(t).at[tok_sorted].add(y_buf[dest] * flat_g[order][:, None].astype(t.dtype))
    return y.reshape(b, s, d)


def setup_inputs(seed: int = 0) -> dict:
    key = jax.random.key(seed)
    ks = jax.random.split(key, 20)
    f32 = jnp.float32
    nrm = lambda k, shp, sc: jax.random.normal(k, shp, f32) * sc
    dt0 = jnp.exp(jax.random.uniform(ks[5], (DEPTH, SSD_HEADS), f32)
                  * (math.log(0.1) - math.log(1e-3)) + math.log(1e-3))
    return {
        "x": nrm(ks[0], (BATCH, SEQ, D_MODEL), 1.0),
        "norm_mix": 1.0 + nrm(ks[1], (DEPTH, D_MODEL), 0.02),
        "w_in": nrm(ks[2], (DEPTH, D_MODEL, IN_DIM), D_MODEL ** -0.5),
        "conv_w": nrm(ks[3], (DEPTH, CONV_K, CONV_CH), CONV_K ** -0.5),
        "conv_b": nrm(ks[4], (DEPTH, CONV_CH), 0.02),
        "dt_bias": dt0 + jnp.log(-jnp.expm1(-dt0)),
        "a_log": jnp.log(jax.random.uniform(ks[6], (DEPTH, SSD_HEADS), f32, 1.0, 16.0)),
        "d_skip": 1.0 + nrm(ks[7], (DEPTH, SSD_HEADS), 0.02),
        "ssd_norm": 1.0 + nrm(ks[8], (DEPTH, D_SSD), 0.02),
        "attn_sinks": nrm(ks[9], (DEPTH, ATTN_Q_HEADS), 0.5),
        "attn_norm": 1.0 + nrm(ks[10], (DEPTH, D_ATTN), 0.02),
        "w_out": nrm(ks[11], (DEPTH, D_MIX, D_MODEL), D_MIX ** -0.5),
        "norm_ffn": 1.0 + nrm(ks[12], (DEPTH, D_MODEL), 0.02),
        "w_router_group": nrm(ks[13], (DEPTH, D_MODEL, MOE_GROUPS), D_MODEL ** -0.5),
        "w_router_expert": nrm(ks[14], (DEPTH, D_MODEL, N_EXPERTS), D_MODEL ** -0.5),
        "w_gate": nrm(ks[15], (DEPTH, N_EXPERTS, D_MODEL, D_FF_EXPERT), D_MODEL ** -0.5),
        "w_up": nrm(ks[16], (DEPTH, N_EXPERTS, D_MODEL, D_FF_EXPERT), D_MODEL ** -0.5),
        "w_down": nrm(ks[17], (DEPTH, N_EXPERTS, D_FF_EXPERT, D_MODEL), D_FF_EXPERT ** -0.5),
        "norm_final": 1.0 + nrm(ks[18], (D_MODEL,), 0.02),
    }


def reference(x, norm_mix, w_in, conv_w, conv_b, dt_bias, a_log, d_skip, ssd_norm,
              attn_sinks, attn_norm, w_out, norm_ffn, w_router_group, w_router_expert,
              w_gate, w_up, w_down, norm_final):
    h = x
    for l in range(DEPTH):
        h = h + hybrid_mixer(rmsnorm(h, norm_mix[l]), w_in[l], conv_w[l], conv_b[l],
                             dt_bias[l], a_log[l], d_skip[l], ssd_norm[l],
                             attn_sinks[l], attn_norm[l], w_out[l])
        h = h + hierarchical_moe(rmsnorm(h, norm_ffn[l]), w_router_group[l],
                                 w_router_expert[l], w_gate[l], w_up[l], w_down[l])
    return rmsnorm(h, norm_final)
```

```python
import contextlib
import numpy as np
import ml_dtypes
import concourse.bass as bass
import concourse.mybir as mybir
from concourse.bass_utils import run_bass_kernel_spmd

F32 = mybir.dt.float32
BF16 = mybir.dt.bfloat16
I32 = mybir.dt.int32
AF = mybir.ActivationFunctionType
ALU = mybir.AluOpType
AX = mybir.AxisListType


class Buf:
    __slots__ = ("name", "w", "r", "war")

    def __init__(self, name):
        self.name = name
        self.w = []
        self.r = []
        self.war = []


class DSem:
    __slots__ = ("sem", "count", "name")

    def __init__(self, sem, name):
        self.sem = sem
        self.count = 0
        self.name = name


class Op:
    __slots__ = ("eng", "fn", "deps", "dsem", "dcount", "semval", "needed", "idx")


class Prog:
    ENGS = ("pe", "act", "dve", "pool", "sp")

    def __init__(self, nc, stack, strict_same=False):
        self.nc = nc
        self.stack = stack
        self.strict = strict_same
        self.ops = {e: [] for e in self.ENGS}
        self.esem = {e: stack.enter_context(nc.semaphore("es_" + e)) for e in self.ENGS}
        self.nds = 0
        self.dsems = []

    def dsem(self, name=None):
        self.nds += 1
        name = name or ("ds%d" % self.nds)
        d = DSem(self.stack.enter_context(self.nc.semaphore(name)), name)
        self.dsems.append(d)
        return d

    def barrier(self):
        marks = []
        for e in self.ENGS:
            m = self._record(e, lambda q: q.drain(fusable=False), (), (), (), None)
            m.needed = True
            marks.append(m)
        dm = []
        for d in self.dsems:
            if d.count > 0:
                f = Op()
                f.eng = None
                f.dsem = d
                f.dcount = d.count
                f.needed = False
                f.semval = None
                dm.append(f)
        for e in self.ENGS:
            o = self._record(e, lambda q: q.nop(), (), (), (), None)
            o.deps = [(m, 0) for m in marks if m.eng != e] + [(f, 0) for f in dm]

    def sb(self, name, shape, dt):
        return self.stack.enter_context(self.nc.sbuf_tensor("s_" + name, list(shape), dt))

    def ps(self, name, shape, dt):
        return self.stack.enter_context(self.nc.psum_tensor("p_" + name, list(shape), dt))

    def _record(self, eng, fn, reads, writes, joins, dsem):
        op = Op()
        op.eng = eng
        op.fn = fn
        op.dsem = dsem
        op.needed = False
        op.semval = None
        op.dcount = None
        if dsem is not None:
            dsem.count += 16
            op.dcount = dsem.count
        deps = []
        for b in reads:
            deps.extend((d, 0) for d in b.w)
        for b in writes:
            deps.extend((d, 1) for d in b.w)
            deps.extend((d, 2) for d in b.r)
        for b in joins:
            deps.extend((d, 2) for d in b.war)
            deps.extend((d, 2) for d in b.r)
        op.deps = deps
        for b in reads:
            b.r.append(op)
        for b in writes:
            b.w = [op]
            b.war = b.r
            b.r = []
        for b in joins:
            b.w.append(op)
            if b.r:
                b.war = b.war + b.r
            b.r = []
        op.idx = len(self.ops[eng])
        self.ops[eng].append(op)
        return op

    def op(self, eng, fn, reads=(), writes=(), joins=()):
        return self._record(eng, fn, reads, writes, joins, None)

    def dma(self, eng, dsem, out, in_, reads=(), writes=(), joins=(), **kw):
        return self._record(eng, lambda q: q.dma_start(out=out, in_=in_, **kw),
                            reads, writes, joins, dsem)

    def dma_fn(self, eng, dsem, fn, reads=(), writes=(), joins=()):
        return self._record(eng, fn, reads, writes, joins, dsem)

    def emit(self, final_waits=()):
        nc = self.nc
        for e in self.ENGS:
            for op in self.ops[e]:
                for d, kind in op.deps:
                    if d.dsem is None and (d.eng != e or (self.strict and e != "pe" and kind != 2)):
                        d.needed = True
        for d in final_waits:
            if d.dsem is None:
                d.needed = True
        for e in self.ENGS:
            c = 0
            for op in self.ops[e]:
                if op.dsem is None and op.needed:
                    c += 1
                    op.semval = c
        stats = {}

        def run(e, q):
            waited = {}
            nw = 0
            ops = self.ops[e]
            for op in ops:
                need = {}
                for d, kind in op.deps:
                    if d.dsem is not None:
                        key = ("d", id(d.dsem))
                        sem, val = d.dsem.sem, d.dcount
                    else:
                        if d.eng == e and (not self.strict or e == "pe" or kind == 2):
                            continue
                        key = ("e", d.eng)
                        sem, val = self.esem[d.eng], d.semval
                    if waited.get(key, 0) >= val:
                        continue
                    if key not in need or need[key][1] < val:
                        need[key] = (sem, val)
                for key, (sem, val) in need.items():
                    q.wait_ge(sem, val)
                    waited[key] = val
                    nw += 1
                ins = op.fn(q)
                if op.dsem is not None:
                    ins.then_inc(op.dsem.sem, 16)
                elif op.needed:
                    ins.then_inc(self.esem[e], 1)
            if e == "sp":
                for d in final_waits:
                    if d.dsem is not None:
                        q.wait_ge(d.dsem.sem, d.dcount)
                    else:
                        q.wait_ge(self.esem[d.eng], d.semval)
            stats[e] = (len(ops), nw)

        with nc.Block() as block:
            @block.tensor
            def _(q):
                run("pe", q)

            @block.scalar
            def _(q):
                run("act", q)

            @block.vector
            def _(q):
                run("dve", q)

            @block.gpsimd
            def _(q):
                run("pool", q)

            @block.sync
            def _(q):
                run("sp", q)
        return stats
D = 1024
NWIN = 3984
OZ, OX, OQ, OK_, OV, ODT = 0, 1024, 2560, 3584, 3840, 3968
NEG = -30000.0
BIG = 1.0e4
EPS = 1e-6


def make_consts(NE, CAP):
    i = np.arange(128)
    t = i[:, None]
    l = i[None, :]
    ident = (t == l)
    triinc = (t <= l)
    ones = np.ones((128, 128))
    U = (t > l)
    slt = (t < l)
    negprev = np.where(t > l, 0.0, NEG)
    negcur = np.where(t <= l, 0.0, NEG)
    ecap = np.broadcast_to((np.arange(NE) * CAP)[None, :], (128, NE))
    c32 = np.concatenate([ident, triinc, ones, U, ecap], axis=1)
    c16 = np.concatenate([ident, slt, ones] + [negprev] * 4 + [negcur] * 4 + [triinc], axis=1)
    return np.ascontiguousarray(c32.astype(np.float32)), np.ascontiguousarray(c16.astype(np.float32))


C_ID, C_TRI, C_ONE, C_U, C_EC = [k * 128 for k in range(5)]
NC16 = 3 * 128 + 8 * 128 + 128


def build_nc(S, NG, EPG, CAP, stop=0):
    NCH = S // 128
    NE = NG * EPG
    NR = NG + NE
    NSLOT = NE * CAP
    NBLK = CAP // 128
    CW = 4 * 128 + NE
    P = 128
    nc = bass.Bass("TRN2", target_bir_lowering=False)
    din = lambda n, s, dt=F32: nc.dram_tensor(n, list(s), dt, kind="ExternalInput").ap()
    x_d = din("x", [S, D])
    win_d = din("win", [D, NWIN])
    wout_d = din("wout", [2 * D, D])
    cw_d = din("cw", [P, 48])
    cb_d = din("cb", [1, 1536])
    sv_d = din("sv", [P, 64])
    gfin_d = din("gfin", [P, D])
    gT_d = din("gT", [P, 32])
    wr_d = din("wr", [D, NR])
    wg_d = din("wg", [NE, D, 512])
    wu_d = din("wu", [NE, D, 512])
    wd_d = din("wd", [NE, 512, D])
    cst_d = din("cst", [P, CW])
    cstb_d = din("cstb", [P, NC16])
    out_d = nc.dram_tensor("out", [S, D], F32, kind="ExternalOutput").ap()
    h1_d = nc.dram_tensor("h1s", [S, D], F32).ap()
    buf_d = nc.dram_tensor("bufs", [NSLOT + S, D], BF16).ap()
    ybuf_d = nc.dram_tensor("ybufs", [NSLOT + S, D], F32).ap()

    with contextlib.ExitStack() as st:
        p = Prog(nc, st, strict_same=True)
        sb = p.sb

        def T(name, shape, dt):
            return sb(name, shape, dt), Buf(name)

        UN = 8 * NWIN + 16 * D
        big = sb("big", [P, UN], BF16)
        b_big = Buf("big")
        win = big[:, 0:8 * NWIN].rearrange("p (j n) -> p j n", j=8)
        wout = big[:, 8 * NWIN:UN].rearrange("p (j n) -> p j n", j=16)
        cst, b_cst = T("cst", [P, CW], F32)
        cb16, b_cb16 = T("cb16", [P, NC16], BF16)
        identb = cb16[:, 0:128]
        sltb = cb16[:, 128:256]
        onesb = cb16[:, 256:384]
        negp = cb16[:, 384:896]
        negc = cb16[:, 896:1408]
        tri16 = cb16[:, 1408:1536]
        b_identb = b_sltb = b_onesb = b_negp = b_negc = b_cb16
        cwt, b_cwt = T("cwt", [P, 48], F32)
        cdiag, b_cdiag = T("cdiag", [P, 48, P], BF16)
        cbb, b_cbb = T("cbb", [1, 1536], BF16)
        sv, b_sv = T("sv", [P, 64], F32)
        aneg, b_aneg = T("aneg", [P, 16], F32)
        esink, b_esink = T("esink", [P, 16], F32)
        gT, b_gT = T("gT", [P, 32], F32)
        wr, b_wr = T("wr", [P, 8, NR], F32)
        epst, b_epst = T("epst", [P, 2], F32)
        S32, b_S32 = T("S32", [P, D], F32)
        Sbf, b_Sbf = T("Sbf", [P, D], BF16)
        mcum, b_mcum = T("mcum", [P, NE], BF16)
        dest_all, b_dest = T("dest_all", [P, NCH * 2], I32)
        gate_all, b_gate = T("gate_all", [P, NCH * 2], F32)
        xbcraw, b_xbcraw = T("xbcraw", [P, 12, 132], BF16)
        xbcrawB, b_xbcrawB = T("xbcrawB", [P, 12, 132], BF16)
        AW = 19820
        arena = sb("arena", [P, AW], F32)
        aoff = [0]

        def carve(name, shape, dt):
            n = 1
            for d_ in shape[1:]:
                n *= d_
            nb = n * (4 if dt in (F32, I32) else 2)
            nw = (nb + 3) // 4
            a = aoff[0]
            aoff[0] += nw
            assert aoff[0] <= AW, ("arena overflow", name, aoff[0])
            v = arena[:, a:a + nw]
            if dt != F32:
                v = v.bitcast(dt)
            v = v[:, 0:n]
            if len(shape) == 3:
                v = v.rearrange("p (a b) -> p a b", a=shape[1])
            return v, Buf(name)

        class Rot:
            def __init__(self, tiles):
                self.t = tiles
                self.i = 0

            def next(self):
                r = self.t[self.i]
                self.i = (self.i + 1) % len(self.t)
                return r

        pbs = Rot([(p.ps("pb%d" % i, [P, 512], F32), Buf("pb%d" % i)) for i in range(6)])
        pts = Rot([(p.ps("pt%d" % i, [P, 1024], BF16), Buf("pt%d" % i)) for i in range(2)])

        def mm(out, lhsT, rhs, first, last, reads, bank, newgrp=None):
            new = first if newgrp is None else newgrp
            kw = dict(writes=[bank]) if new else dict(joins=[bank])
            p.op("pe", lambda q: q.matmul(out=out, lhsT=lhsT, rhs=rhs, start=first, stop=last), reads=reads, **kw)

        def tr(out, in_, ident, reads, bank, new):
            kw = dict(writes=[bank]) if new else dict(joins=[bank])
            p.op("pe", lambda q: q.transpose(out=out, in_=in_, identity=ident), reads=reads, **kw)

        def act(out, in_, func, reads, writes=(), joins=(), **kw):
            p.op("act", lambda q: q.activation(out=out, in_=in_, func=func, **kw), reads=reads, writes=writes, joins=joins)

        def tt(eng, out, in0, in1, op, reads, writes=(), joins=()):
            p.op(eng, lambda q: q.tensor_tensor(out=out, in0=in0, in1=in1, op=op), reads=reads, writes=writes, joins=joins)

        def ts(eng, out, in0, s1, op0, reads, writes=(), joins=(), s2=None, op1=None):
            if op1 is None:
                p.op(eng, lambda q: q.tensor_scalar(out=out, in0=in0, scalar1=s1, scalar2=None, op0=op0),
                     reads=reads, writes=writes, joins=joins)
            else:
                p.op(eng, lambda q: q.tensor_scalar(out=out, in0=in0, scalar1=s1, scalar2=s2, op0=op0, op1=op1),
                     reads=reads, writes=writes, joins=joins)

        def stt(out, in0, scalar, in1, op0, op1, reads, writes=(), joins=()):
            p.op("dve", lambda q: q.scalar_tensor_tensor(out=out, in0=in0, scalar=scalar, in1=in1, op0=op0, op1=op1),
                 reads=reads, writes=writes, joins=joins)

        def cp(eng, out, in_, reads, writes=(), joins=()):
            if eng == "act":
                act(out, in_, AF.Copy, reads, writes, joins)
            else:
                p.op(eng, lambda q: q.tensor_copy(out=out, in_=in_), reads=reads, writes=writes, joins=joins)

        def rstd_from_ss(ss_ap, rs_ap, n, b_ss, b_rs):
            act(rs_ap, ss_ap, AF.Ln, [b_ss, b_epst], writes=[b_rs], bias=epst[:, 0:1], scale=1.0 / n)
            act(rs_ap, rs_ap, AF.Exp, [b_rs], writes=[b_rs], scale=-0.5)

        dI = [p.dsem() for _ in range(12)]
        p.dma("sp", dI[0], cst[:], cst_d, writes=[b_cst])
        p.dma("sp", dI[1], cwt[:], cw_d, writes=[b_cwt])
        p.dma("pool", dI[2], cbb[:], cb_d, writes=[b_cbb])
        p.dma("sp", dI[3], sv[:], sv_d, writes=[b_sv])
        p.dma("sp", dI[5], gT[:], gT_d, writes=[b_gT])
        p.dma("sp", dI[6], wr[:], wr_d.rearrange("(j p) n -> p j n", p=P), writes=[b_wr])
        b_wout = Buf("wout")
        for j in range(8):
            p.dma("pool", dI[7], win[:, j, :], win_d[j * P:(j + 1) * P, :], **(dict(writes=[b_big]) if j == 0 else dict(joins=[b_big])))
        for j in range(16):
            p.dma("pool", dI[9], wout[:, j, :], wout_d[j * P:(j + 1) * P, :], **(dict(writes=[b_wout]) if j == 0 else dict(joins=[b_wout])))
        p.op("pool", lambda q: q.memset(epst[:, 0:1], EPS), writes=[b_epst])
        p.op("pool", lambda q: q.memset(epst[:, 1:2], 1.0), joins=[b_epst])
        p.op("pool", lambda q: q.memset(S32[:], 0.0), writes=[b_S32])
        p.op("pool", lambda q: q.memset(Sbf[:], 0.0), writes=[b_Sbf])
        p.op("pool", lambda q: q.memset(mcum[:], 0.0), writes=[b_mcum])
        p.op("pool", lambda q: q.memset(xbcraw[:], 0.0), writes=[b_xbcraw])
        p.op("pool", lambda q: q.memset(xbcrawB[:], 0.0), writes=[b_xbcrawB])
        p.dma("pool", dI[8], cb16[:], cstb_d, writes=[b_cb16])
        tt("pool", cdiag[:], identb.unsqueeze(1).to_broadcast([P, 48, P]),
           cwt[:].unsqueeze(2).to_broadcast([P, 48, P]), ALU.mult, [b_identb, b_cwt], writes=[b_cdiag])
        act(aneg[:], sv[:, 16:32], AF.Exp, [b_sv], writes=[b_aneg])
        ts("pool", aneg[:], aneg[:], -1.0, ALU.mult, [b_aneg], writes=[b_aneg])
        act(esink[:], sv[:, 48:64], AF.Exp, [b_sv], writes=[b_esink])
        ident32 = cst[:, C_ID:C_ID + P]
        tri32 = cst[:, C_TRI:C_TRI + P]
        ones32 = cst[:, C_ONE:C_ONE + P]
        U32 = cst[:, C_U:C_U + P]

        def slots(name, n, shape, dt):
            return [carve("%s%d" % (name, i), shape, dt) for i in range(n)]

        xc = slots("xc", 1, [P, D], F32) * 2
        dx = [p.dsem()] * 2
        hTb = slots("hTb", 1, [P, 8, P], F32)
        dxo = p.dsem()
        stF = slots("stF", 2, [P, 2], F32)
        stS = slots("stS", 1, [P, 4], F32)
        stA = slots("stA", 1, [P, 2], F32)
        stT = slots("stT", 1, [P, 2], F32)
        aden = slots("aden", 1, [P, 16], F32)
        xn = slots("xn", 1, [P, D], BF16)
        junk, b_junk = carve("junk", [P, D], BF16)
        xnT = slots("xnT", 1, [P, 8, P], BF16)
        sz = slots("sz", 2, [P, D], BF16)
        xbcT = slots("xbcT", 2, [P, 12, P], BF16)
        qT = slots("qT", 2, [P, 8, P], BF16)
        kT = slots("kT", 3, [P, 2, P], BF16)
        Vp = slots("Vp", 3, [P, 2, 66], BF16)
        dts = slots("dts", 2, [P, 16 * 8], F32)
        xs_tok = slots("xs_tok", 1, [P, D], BF16)
        B_tok = slots("B_tok", 1, [P, 2 * P], BF16)
        xdt = slots("xdt", 1, [P, D], BF16)
        xdt2 = slots("xdt2", 1, [P, D], BF16)
        segLh = slots("segLh", 1, [P, 8, P], BF16)
        segLl = slots("segLl", 1, [P, 8, P], BF16)
        ahl = slots("ahl", 1, [P, 32], BF16)
        alo32 = slots("alo32", 1, [P, 16], F32)
        dec = slots("dec", 2, [P, 4, P], BF16)
        MT = slots("MT", 1, [P, 16, P], BF16)
        cbm = slots("cbm", 1, [P, 2, P], BF16)
        ya = slots("ya", 1, [P, D], F32)
        uu = slots("uu", 1, [P, D], F32)
        PT = [[carve("PT%d_%d" % (i, k), [P, 4, P], BF16) for k in range(2)] for i in range(2)]
        attraw = slots("attraw", 1, [P, 16, 65], F32)
        mixed = slots("mixed", 1, [P, 2 * D], BF16)
        mixT = slots("mixT", 1, [P, 16, P], BF16)
        dh1 = [p.dsem() for _ in range(2)]
        hnb = slots("hnb", 1, [P, D], BF16)
        dsc = [p.dsem() for _ in range(2)]
        rt = slots("rt", 1, [P, NR + 16 + 3 * NG + 7 * NE], F32)
        mselb = slots("mselb", 1, [P, NE], BF16)
        b_h1d = Buf("h1_d")
        b_bufd = Buf("buf_d")
        b_bufz = Buf("buf_z")
        b_ybufd = Buf("ybuf_d")
        for i in range(3):
            p.op("pool", lambda q, i=i: q.memset(Vp[i][0][:], 1.0), writes=[Vp[i][1]])

        def wj(first, b):
            return dict(writes=[b]) if first else dict(joins=[b])

        _mxseen = set()

        def mxfirst(c):
            if c in _mxseen:
                return False
            _mxseen.add(c)
            return True

        def front_pro(c):
            if c >= NCH:
                return
            s2 = c % 2
            xct, b_xc = xc[s2]
            stt_, b_st = stF[s2]
            p.dma("sp", dx[s2], xct[:], x_d[c * P:(c + 1) * P, :], writes=[b_xc])
            act(junk[:], xct[:], AF.Square, [b_xc], writes=[b_st, b_junk], accum_out=stt_[:, 0:1])
            rstd_from_ss(stt_[:, 0:1], stt_[:, 1:2], D, b_st, b_st)
            xnt, b_xn = xn[0]
            ts("dve", xnt[:], xct[:], stt_[:, 1:2], ALU.mult, [b_xc, b_st], writes=[b_xn])

        def front(c):
            s2, s3 = c % 2, c % 3
            xnt, b_xn = xn[0]
            pt, b_pt = pts.next()
            for j in range(8):
                tr(pt[:, j * P:(j + 1) * P], xnt[:, j * P:(j + 1) * P], identb, [b_xn, b_cb16], b_pt, j == 0)
            xT, b_xT = xnT[0]
            for j in range(8):
                act(xT[:, j, :], pt[:, j * P:(j + 1) * P], AF.Copy, [b_pt, b_gT], scale=gT[:, 24 + j:25 + j], **wj(j == 0, b_xT))
            yield
            kt_, b_kt = kT[s3]
            vp, b_vp = Vp[s3]
            dt_, b_dt = dts[s2]
            pb, b_pb = pbs.next()
            for g in range(2):
                for j in range(8):
                    mm(pb[:, g * P:(g + 1) * P], win[:, j, OK_ + g * P:OK_ + (g + 1) * P], xT[:, j, :],
                       j == 0, j == 7, [b_xT, b_big], b_pb, newgrp=(g == 0 and j == 0))
            for j in range(8):
                mm(pb[:, 256:384], xT[:, j, :], win[:, j, OV:OV + 128], j == 0, j == 7, [b_xT, b_big], b_pb, newgrp=False)
            for j in range(8):
                mm(pb[:, 384:400], xT[:, j, :], win[:, j, ODT:ODT + 16], j == 0, False, [b_xT, b_big], b_pb, newgrp=False)
            mm(pb[:, 384:400], ones32[0:1, :], sv[0:1, 0:16], False, True, [b_cst, b_sv], b_pb, newgrp=False)
            cp("act", kt_[:], pb[:, 0:256].rearrange("p (g t) -> p g t", g=2), [b_pb], writes=[b_kt])
            cp("act", vp[:, :, 0:64], pb[:, 256:384].rearrange("p (g d) -> p g d", g=2), [b_pb], writes=[b_vp])
            act(dt_[:, 0:16], pb[:, 384:400], AF.Exp, [b_pb], writes=[b_dt])
            act(dt_[:, 16:32], dt_[:, 0:16], AF.Ln, [b_dt, b_epst], writes=[b_dt], bias=epst[:, 1:2])
            tt("dve", dt_[:, 32:48], dt_[:, 16:32], aneg[:], ALU.mult, [b_dt, b_aneg], writes=[b_dt])
            front_pro(c + 1)
            yield
            szt, b_sz = sz[s2]
            for half in range(2):
                pb, b_pb = pbs.next()
                for j in range(8):
                    mm(pb[:, :], xT[:, j, :], win[:, j, OZ + half * 512:OZ + (half + 1) * 512], j == 0, j == 7, [b_xT, b_big], b_pb)
                act(szt[:, half * 512:(half + 1) * 512], pb[:, :], AF.Silu, [b_pb], **wj(half == 0, b_sz))
            yield
            for grp in range(3):
                pb, b_pb = pbs.next()
                for mm_ in range(4):
                    m = grp * 4 + mm_
                    for j in range(8):
                        mm(pb[:, mm_ * P:(mm_ + 1) * P], win[:, j, OX + m * P:OX + (m + 1) * P], xT[:, j, :],
                           j == 0, j == 7, [b_xT, b_big], b_pb, newgrp=(mm_ == 0 and j == 0))
                cp("act", xbcraw[:, grp * 4:(grp + 1) * 4, 4:132], pb[:, :].rearrange("p (a t) -> p a t", a=4), [b_pb],
                   **wj(grp == 0, b_xbcraw))
            yield
            p.dma("sp", dxo, xbcrawB[:, :, 0:131], xbcraw[:, :, 1:132], reads=[b_xbcraw], writes=[b_xbcrawB])
            xbt, b_xbt = xbcT[s2]
            for grp in range(3):
                pb, b_pb = pbs.next()
                for mm_ in range(4):
                    m = grp * 4 + mm_
                    for k in range(4):
                        src_, b_src = (xbcraw, b_xbcraw) if k % 2 == 1 else (xbcrawB, b_xbcrawB)
                        o_ = k + 1 if k % 2 == 1 else k
                        mm(pb[:, mm_ * P:(mm_ + 1) * P], cdiag[:, m * 4 + k, :], src_[:, m, o_:o_ + P],
                           k == 0, False, [b_cdiag, b_src], b_pb, newgrp=(mm_ == 0 and k == 0))
                    mm(pb[:, mm_ * P:(mm_ + 1) * P], cbb[0:1, m * P:(m + 1) * P], onesb[0:1, :], False, True,
                       [b_cbb, b_cb16], b_pb, newgrp=False)
                act(xbt[:, grp * 4:(grp + 1) * 4, :], pb[:, :].rearrange("p (a t) -> p a t", a=4), AF.Silu, [b_pb],
                    **wj(grp == 0, b_xbt))
                if grp == 2:
                    cp("pool", xbcraw[:, :, 0:4], xbcraw[:, :, 128:132], [b_xbcraw], writes=[b_xbcraw])
            yield
            qt, b_qt = qT[s2]
            for grp in range(2):
                pb, b_pb = pbs.next()
                for mm_ in range(4):
                    m = grp * 4 + mm_
                    for j in range(8):
                        mm(pb[:, mm_ * P:(mm_ + 1) * P], win[:, j, OQ + m * P:OQ + (m + 1) * P], xT[:, j, :],
                           j == 0, j == 7, [b_xT, b_big], b_pb, newgrp=(mm_ == 0 and j == 0))
                cp("dve", qt[:, grp * 4:(grp + 1) * 4, :], pb[:, :].rearrange("p (a t) -> p a t", a=4), [b_pb],
                   **wj(grp == 0, b_qt))
            yield
        def ssd(c):
            s2 = c % 2
            stt_, b_st = stS[0]
            xbt, b_xbt = xbcT[s2]
            szt, b_sz = sz[s2]
            dt_, b_dt = dts[s2]
            dtv = dt_[:, 16:32]
            av = dt_[:, 32:48]
            ev = dt_[:, 96:112]
            cdv = dt_[:, 112:128]
            xst, b_xst = xs_tok[0]
            Bt, b_Bt = B_tok[0]
            xd, b_xd = xdt[0]
            xd2, b_xd2 = xdt2[0]
            cb_, b_cb = cbm[0]
            sLh, b_sL = segLh[0]
            sLl = segLl[0][0]
            ah, b_ah = ahl[0]
            al32, b_al = alo32[0]
            mt, b_mt = MT[0]
            yat, b_ya = ya[0]
            ut, b_u = uu[0]
            mx, b_mx = mixed[0]

            def seg_heads(hq):
                pb, b_pb = pbs.next()
                for hh in range(4):
                    i_ = (hq % 2) * 4 + hh
                    mm(pb[:, hh * P:(hh + 1) * P], sLh[:, i_, :], tri16, True, False, [b_sL, b_cb16], b_pb, newgrp=(hh == 0))
                    mm(pb[:, hh * P:(hh + 1) * P], sLl[:, i_, :], tri16, False, True, [b_sL, b_cb16], b_pb, newgrp=False)
                dc, b_dc = dec[hq % 2]
                act(dc[:], pb[:, :].rearrange("p (a t) -> p a t", a=4), AF.Exp, [b_pb], writes=[b_dc])
                tt("dve", mt[:, hq * 4:(hq + 1) * 4, :], dc[:],
                   cb_[:, hq // 2, :].unsqueeze(1).to_broadcast([P, 4, P]), ALU.mult, [b_dc, b_cb], **wj(hq == 0, b_mt))

            def seg_build(h0):
                tt("pool", sLh[:], U32.unsqueeze(1).to_broadcast([P, 8, P]),
                   ah[:, h0:h0 + 8].unsqueeze(2).to_broadcast([P, 8, P]), ALU.mult, [b_cst, b_ah], writes=[b_sL])
                tt("pool", sLl[:], U32.unsqueeze(1).to_broadcast([P, 8, P]),
                   ah[:, 16 + h0:16 + h0 + 8].unsqueeze(2).to_broadcast([P, 8, P]), ALU.mult, [b_cst, b_ah], joins=[b_sL])

            cp("dve", ah[:, 0:16], av, [b_dt], writes=[b_ah])
            tt("dve", al32[:], av, ah[:, 0:16], ALU.subtract, [b_dt, b_ah], writes=[b_al])
            cp("dve", ah[:, 16:32], al32[:], [b_al], joins=[b_ah])
            seg_build(0)
            pb, b_pb = pbs.next()
            mm(pb[:, 0:16], tri32, av, True, True, [b_cst, b_dt], b_pb)
            mm(pb[:, 16:32], ones32, av, True, True, [b_cst, b_dt], b_pb, newgrp=False)
            cp("act", dt_[:, 48:80], pb[:, 0:32], [b_pb], writes=[b_dt])
            pt, b_pt = pts.next()
            for j in range(8):
                tr(pt[:, j * P:(j + 1) * P], xbt[:, j, :], identb, [b_xbt, b_cb16], b_pt, j == 0)
            cp("act", xst[:], pt[:], [b_pt], writes=[b_xst])
            pt, b_pt = pts.next()
            for g in range(2):
                tr(pt[:, g * P:(g + 1) * P], xbt[:, 8 + g, :], identb, [b_xbt, b_cb16], b_pt, g == 0)
            cp("dve", Bt[:], pt[:, 0:2 * P], [b_pt], writes=[b_Bt])
            pb, b_pb = pbs.next()
            for g in range(2):
                mm(pb[:, g * P:(g + 1) * P], xbt[:, 8 + g, :], xbt[:, 10 + g, :], True, True, [b_xbt], b_pb, newgrp=(g == 0))
            tt("dve", cb_[:], pb[:, 0:256].rearrange("p (g t) -> p g t", g=2), tri32.unsqueeze(1).to_broadcast([P, 2, P]),
               ALU.mult, [b_pb, b_cst], writes=[b_cb])
            tt("pool", dt_[:, 80:96], dt_[:, 64:80], dt_[:, 48:64], ALU.subtract, [b_dt], writes=[b_dt])
            act(dt_[:, 96:112], dt_[:, 48:64], AF.Exp, [b_dt], writes=[b_dt])
            act(dt_[:, 80:96], dt_[:, 80:96], AF.Exp, [b_dt], writes=[b_dt])
            act(dt_[:, 112:128], dt_[:, 64:80], AF.Exp, [b_dt], writes=[b_dt])
            tt("pool", dt_[:, 0:16], dtv, dt_[:, 80:96], ALU.mult, [b_dt], writes=[b_dt])
            tt("dve", xd[:].rearrange("p (h d) -> p h d", h=16), xst[:].rearrange("p (h d) -> p h d", h=16),
               dtv.unsqueeze(2).to_broadcast([P, 16, 64]), ALU.mult, [b_xst, b_dt], writes=[b_xd])
            tt("pool", xd2[:].rearrange("p (h d) -> p h d", h=16), xst[:].rearrange("p (h d) -> p h d", h=16),
               dt_[:, 0:16].unsqueeze(2).to_broadcast([P, 16, 64]), ALU.mult, [b_xst, b_dt], writes=[b_xd2])
            yield
            seg_heads(0)
            seg_heads(1)
            seg_build(8)
            pbo = [pbs.next() for _ in range(2)]
            for g in range(2):
                mm(pbo[g][0][:, :], xbt[:, 10 + g, :], Sbf[:, g * 512:(g + 1) * 512], True, True, [b_xbt, b_Sbf], pbo[g][1])
            for g in range(2):
                tt("dve", yat[:, g * 512:(g + 1) * 512].rearrange("p (h d) -> p h d", h=8),
                   pbo[g][0][:, :].rearrange("p (h d) -> p h d", h=8),
                   ev[:, g * 8:(g + 1) * 8].unsqueeze(2).to_broadcast([P, 8, 64]), ALU.mult, [pbo[g][1], b_dt], **wj(g == 0, b_ya))
            yield
            seg_heads(2)
            seg_heads(3)
            pbn = [pbs.next() for _ in range(2)]
            for g in range(2):
                mm(pbn[g][0][:, :], Bt[:, g * P:(g + 1) * P], xd2[:, g * 512:(g + 1) * 512], True, True, [b_Bt, b_xd2], pbn[g][1])
            tt("pool", S32[:].rearrange("p (h d) -> p h d", h=16), S32[:].rearrange("p (h d) -> p h d", h=16),
               cdv.unsqueeze(2).to_broadcast([P, 16, 64]), ALU.mult, [b_S32, b_dt], writes=[b_S32])
            for g in range(2):
                tt("dve", S32[:, g * 512:(g + 1) * 512], S32[:, g * 512:(g + 1) * 512], pbn[g][0][:, :], ALU.add,
                   [b_S32, pbn[g][1]], **wj(g == 0, b_S32))
            cp("pool", Sbf[:], S32[:], [b_S32], writes=[b_Sbf])
            yield
            pbd = [pbs.next() for _ in range(2)]
            for h in range(16):
                g = h // 8
                mm(pbd[g][0][:, (h % 8) * 64:(h % 8 + 1) * 64], mt[:, h, :], xd[:, h * 64:(h + 1) * 64], True, True,
                   [b_mt, b_xd], pbd[g][1], newgrp=(h % 8 == 0))
            for g in range(2):
                tt("dve", yat[:, g * 512:(g + 1) * 512], yat[:, g * 512:(g + 1) * 512], pbd[g][0][:, :], ALU.add,
                   [b_ya, pbd[g][1]], **wj(g == 0, b_ya))
            tt("pool", ut[:].rearrange("p (h d) -> p h d", h=16), xst[:].rearrange("p (h d) -> p h d", h=16),
               sv[:, 32:48].unsqueeze(2).to_broadcast([P, 16, 64]), ALU.mult, [b_xst, b_sv], writes=[b_u])
            tt("pool", yat[:], yat[:], ut[:], ALU.add, [b_u, b_ya], writes=[b_ya])
            tt("pool", ut[:], yat[:], szt[:], ALU.mult, [b_ya, b_sz], writes=[b_u])
            yield
            for g in range(2):
                act(junk[:, 0:512], ut[:, g * 512:(g + 1) * 512], AF.Square, [b_u], writes=[b_st, b_junk],
                    accum_out=stt_[:, g:g + 1])
            rstd_from_ss(stt_[:, 0:2], stt_[:, 2:4], 512, b_st, b_st)
            for g in range(2):
                ts("dve", mx[:, g * 512:(g + 1) * 512], ut[:, g * 512:(g + 1) * 512], stt_[:, 2 + g:3 + g], ALU.mult,
                   [b_u, b_st], **wj(mxfirst(c), b_mx))
            yield

        def attn(c):
            s2, s3 = c % 2, c % 3
            stt_, b_st = stA[0]
            qt, b_qt = qT[s2]
            ar, b_ar = attraw[0]
            first_ar = True
            kts = ([(c - 1) % 3] if c > 0 else []) + [s3]
            for g in range(2):
                for par in range(2):
                    for ki, ks in enumerate(kts):
                        isprev = (c > 0 and ki == 0)
                        pb, b_pb = pbs.next()
                        ktt, b_ktt = kT[ks]
                        mm(pb[:, :], ktt[par * 64:(par + 1) * 64, g, :], qt[par * 64:(par + 1) * 64, g * 4:(g + 1) * 4, :],
                           True, False, [b_ktt, b_qt], b_pb)
                        mm(pb[:, :], identb, negp if isprev else negc, False, True, [b_cb16], b_pb, newgrp=False)
                        ptile, b_ptile = PT[par][0 if isprev else 1]
                        act(ptile[:], pb[:, :].rearrange("p (a t) -> p a t", a=4), AF.Exp, [b_pb], writes=[b_ptile], scale=0.125)
                    pb, b_pb = pbs.next()
                    for t4 in range(4):
                        for ki, ks in enumerate(kts):
                            isprev = (c > 0 and ki == 0)
                            ptile, b_ptile = PT[par][0 if isprev else 1]
                            vpt, b_vpt = Vp[ks]
                            mm(pb[:, t4 * 65:(t4 + 1) * 65], ptile[:, t4, :], vpt[:, g, 0:65], ki == 0, ki == len(kts) - 1,
                               [b_ptile, b_vpt], b_pb, newgrp=(t4 == 0 and ki == 0))
                    cp("act", ar[:, g * 8 + par:g * 8 + 8:2, :], pb[:, 0:260].rearrange("p (a t) -> p a t", a=4), [b_pb],
                       **wj(first_ar, b_ar))
                    first_ar = False
                    yield
            dn, b_dn = aden[0]
            tt("dve", dn[:], ar[:, :, 64], esink[:], ALU.add, [b_ar, b_esink], writes=[b_dn])
            p.op("dve", lambda q: q.reciprocal(out=dn[:], in_=dn[:]), reads=[b_dn], writes=[b_dn])
            at3 = ar[:, :, 0:64]
            tt("dve", at3, at3, dn[:].unsqueeze(2).to_broadcast([P, 16, 64]), ALU.mult, [b_ar, b_dn], writes=[b_ar])
            act(junk[:].rearrange("p (h d) -> p h d", h=16), at3, AF.Square, [b_ar], writes=[b_st, b_junk], accum_out=stt_[:, 0:1])
            rstd_from_ss(stt_[:, 0:1], stt_[:, 1:2], D, b_st, b_st)
            mx, b_mx = mixed[0]
            ts("dve", mx[:, D:2 * D].rearrange("p (h d) -> p h d", h=16), at3, stt_[:, 1:2], ALU.mult, [b_ar, b_st], **wj(mxfirst(c), b_mx))
            yield

        dxr = p.dsem()

        def tail(c):
            s2 = c % 2
            mx, b_mx = mixed[0]
            mxT, b_mxT = mixT[0]
            ut, b_u = uu[0]
            p.dma("sp", dxr, ut[:], x_d[c * P:(c + 1) * P, :], writes=[b_u])
            for hb in range(2):
                pt, b_pt = pts.next()
                for j in range(8):
                    tr(pt[:, j * P:(j + 1) * P], mx[:, (hb * 8 + j) * P:(hb * 8 + j + 1) * P], identb, [b_mx, b_cb16], b_pt, j == 0)
                tt("dve", mxT[:, hb * 8:(hb + 1) * 8, :], pt[:].rearrange("p (j t) -> p j t", j=8),
                   gT[:, hb * 8:(hb + 1) * 8].unsqueeze(2).to_broadcast([P, 8, P]), ALU.mult, [b_pt, b_gT], **wj(hb == 0, b_mxT))
            if (c - 1) in pending:
                pending.pop(c - 1)()
            yield
            for half in range(2):
                pb, b_pb = pbs.next()
                for j in range(16):
                    mm(pb[:, :], mxT[:, j, :], wout[:, j, half * 512:(half + 1) * 512], j == 0, j == 15, [b_mxT, b_wout], b_pb)
                tt("dve", ut[:, half * 512:(half + 1) * 512], pb[:, :], ut[:, half * 512:(half + 1) * 512], ALU.add,
                   [b_pb, b_u], writes=[b_u])
                if half == 1:
                    f_ = p.dma("sp", dh1[s2], (out_d if stop else h1_d)[c * P:(c + 1) * P, :], ut[:], reads=[b_u], joins=[b_h1d])
                    finals1.append(f_)
            yield

        pending = {}

        def trt(c):
            yield from tail(c)
            if not stop:
                yield from rtail(c)

        def rtail(c):
            stt_, b_st = stT[0]
            ut, b_u = uu[0]
            act(junk[:], ut[:], AF.Square, [b_u], writes=[b_st, b_junk], accum_out=stt_[:, 0:1])
            rstd_from_ss(stt_[:, 0:1], stt_[:, 1:2], D, b_st, b_st)
            ts("dve", ut[:], ut[:], stt_[:, 1:2], ALU.mult, [b_u, b_st], writes=[b_u])
            hb_, b_hb = hnb[0]
            cp("act", hb_[:], ut[:], [b_u], writes=[b_hb])
            hT, b_hT = hTb[0]
            for hf in range(2):
                pb, b_pb = pbs.next()
                for j in range(4):
                    tr(pb[:, j * P:(j + 1) * P], ut[:, (hf * 4 + j) * P:(hf * 4 + j + 1) * P], ident32, [b_u, b_cst], b_pb, j == 0)
                tt("dve", hT[:, hf * 4:(hf + 1) * 4, :], pb[:, :].rearrange("p (j t) -> p j t", j=4),
                   gT[:, 16 + hf * 4:16 + (hf + 1) * 4].unsqueeze(2).to_broadcast([P, 4, P]), ALU.mult, [b_pb, b_gT],
                   **wj(hf == 0, b_hT))
            yield
            pb, b_pb = pbs.next()
            for j in range(8):
                mm(pb[:, 0:NR], hT[:, j, :], wr[:, j, :], j == 0, j == 7, [b_hT, b_wr], b_pb)
            r, b_r = rt[0]
            o = [0]

            def seg(n):
                a = r[:, o[0]:o[0] + n]
                o[0] += n
                return a

            lg = seg(NR)
            gmax, ngmax, gsum, pgrp, m1, m2, dd, ex, den, g1, g2, d1, d2 = [seg(1) for _ in range(13)]
            goh, pen, gex = seg(NG), seg(NG), seg(NG)
            ml, oh1, ml2, oh2, msel, rk, tmp = [seg(NE) for _ in range(7)]
            cp("act", lg, pb[:, 0:NR], [b_pb], writes=[b_r])
            yield
            R = [b_r]
            W = dict(writes=[b_r])
            p.op("dve", lambda q: q.tensor_reduce(out=gmax, in_=lg[:, 0:NG], axis=AX.X, op=ALU.max), reads=R, **W)
            ts("dve", goh, lg[:, 0:NG], gmax, ALU.is_equal, R, **W)
            ts("dve", ngmax, gmax, -1.0, ALU.mult, R, **W)
            act(gex, lg[:, 0:NG], AF.Exp, R, bias=ngmax, accum_out=gsum, **W)
            p.op("dve", lambda q: q.reciprocal(out=pgrp, in_=gsum), reads=R, **W)
            ts("dve", pen, goh, -1.0, ALU.add, R, s2=BIG, op1=ALU.mult, **W)
            tt("dve", ml.rearrange("p (g e) -> p g e", g=NG), lg[:, NG:NR].rearrange("p (g e) -> p g e", g=NG),
               pen.unsqueeze(2).to_broadcast([P, NG, EPG]), ALU.add, R, **W)
            p.op("dve", lambda q: q.tensor_reduce(out=m1, in_=ml, axis=AX.X, op=ALU.max), reads=R, **W)
            ts("dve", oh1, ml, m1, ALU.is_equal, R, **W)
            stt(ml2, oh1, -BIG, ml, ALU.mult, ALU.add, R, **W)
            p.op("dve", lambda q: q.tensor_reduce(out=m2, in_=ml2, axis=AX.X, op=ALU.max), reads=R, **W)
            ts("dve", oh2, ml2, m2, ALU.is_equal, R, **W)
            yield
            tt("dve", dd, m2, m1, ALU.subtract, R, **W)
            act(ex, dd, AF.Exp, R, **W)
            ts("dve", den, ex, 1.0, ALU.add, R, **W)
            p.op("dve", lambda q: q.reciprocal(out=g1, in_=den), reads=R, **W)
            tt("dve", g2, ex, g1, ALU.mult, R, **W)
            tt("dve", gate_all[:, 2 * c:2 * c + 1], g1, pgrp, ALU.mult, R, joins=[b_gate])
            tt("dve", gate_all[:, 2 * c + 1:2 * c + 2], g2, pgrp, ALU.mult, R, joins=[b_gate])
            tt("dve", msel, oh1, oh2, ALU.add, R, **W)
            msb, b_msb = mselb[0]
            cp("dve", msb[:], msel, R, writes=[b_msb])
            def rank_and_scatter():
                pb, b_pb = pbs.next()
                mm(pb[:, 0:NE], sltb, msb[:], True, False, [b_cb16, b_msb], b_pb)
                mm(pb[:, 0:NE], onesb, mcum[:], False, True, [b_cb16, b_mcum], b_pb, newgrp=False)
                tt("dve", mcum[:], mcum[:], msb[:], ALU.add, [b_mcum, b_msb], writes=[b_mcum])
                tt("dve", rk, pb[:, 0:NE], cst[:, C_EC:C_EC + NE], ALU.add, [b_pb, b_cst], **W)
                for k, (oh, dk) in enumerate(((oh1, d1), (oh2, d2))):
                    tt("dve", tmp, oh, rk, ALU.mult, R, **W)
                    p.op("dve", lambda q, dk=dk: q.tensor_reduce(out=dk, in_=tmp, axis=AX.X, op=ALU.add), reads=R, **W)
                    cp("dve", dest_all[:, 2 * c + k:2 * c + k + 1], dk, R, joins=[b_dest])
                    p.dma_fn("pool", dsc[k], lambda q, k=k, hb_=hb_: q.indirect_dma_start(
                        out=buf_d, out_offset=bass.IndirectOffsetOnAxis(ap=dest_all[:, 2 * c + k:2 * c + k + 1], axis=0),
                        in_=hb_[:, :], in_offset=None),
                        reads=[b_hb, b_dest, b_bufz], joins=[b_bufd])

            pending[c] = rank_and_scatter
            yield

        def run_group(gens):
            gens = [g_ for g_ in gens if g_ is not None]
            while gens:
                for g_ in list(gens):
                    try:
                        next(g_)
                    except StopIteration:
                        gens.remove(g_)

        finals1 = []
        front_pro(0)
        run_group([front(0)])
        if not stop:
            hz, b_hz = hnb[0]
            p.op("pool", lambda q: q.memset(hz[:], 0.0), writes=[b_hz])
            dzr = p.dsem()
            ZK = 16
            assert NSLOT % (P * ZK) == 0
            for zi in range(NSLOT // (P * ZK)):
                p.dma("sp", dzr, buf_d[zi * P * ZK:(zi + 1) * P * ZK, :].rearrange("(p k) d -> p k d", p=P),
                      hz[:].unsqueeze(1).to_broadcast([P, ZK, D]), reads=[b_hz], joins=[b_bufz])
        def delayed(g_, n):
            for _ in range(n):
                yield
            yield from g_

        for c in range(NCH):
            run_group([front(c + 1) if c + 1 < NCH else None, ssd(c), attn(c), trt(c - 1) if c > 0 else None])
        run_group([trt(NCH - 1)])
        if (NCH - 1) in pending:
            pending.pop(NCH - 1)()
        if stop:
            stats = p.emit(final_waits=finals1[-2:])
            return nc, stats

        p.barrier()
        aoff[0] = 0
        NWS = 3
        wgv = [big[:, i * 12288:i * 12288 + 4096].rearrange("p (j n) -> p j n", j=8) for i in range(NWS)]
        wuv = [big[:, i * 12288 + 4096:i * 12288 + 8192].rearrange("p (j n) -> p j n", j=8) for i in range(NWS)]
        wdv = [big[:, i * 12288 + 8192:i * 12288 + 12288].rearrange("p (j n) -> p j n", j=4) for i in range(NWS)]
        b_ws = [Buf("ws%d" % i) for i in range(NWS)]
        dws = [p.dsem() for _ in range(NWS)]
        xe = slots("xe", 4, [P, D], BF16)
        dxe = [p.dsem() for _ in range(4)]
        xeT = slots("xeT", 2, [P, 8, CAP], BF16)
        sg = slots("sg", 2, [P, 4, CAP], BF16)
        hTt = slots("hTt", 2, [P, 4, CAP], BF16)
        yo = slots("yo", 4, [P, D], F32)
        dyo = [p.dsem() for _ in range(4)]
        nx = 0
        ny = 0

        def load_w(e):
            i = e % NWS
            p.dma("pool", dws[i], wgv[i], wg_d[e].rearrange("(j p) n -> p j n", p=P), writes=[b_ws[i]])
            p.dma("pool", dws[i], wuv[i], wu_d[e].rearrange("(j p) n -> p j n", p=P), joins=[b_ws[i]])
            p.dma("pool", dws[i], wdv[i], wd_d[e].rearrange("(j p) n -> p j n", p=P), joins=[b_ws[i]])

        for e in range(min(2, NE)):
            load_w(e)
        def load_xe(e):
            for blk in range(NBLK):
                k_ = (e * NBLK + blk) % 4
                xet, b_xe = xe[k_]
                p.dma("sp", dxe[k_], xet[:], buf_d[e * CAP + blk * P:e * CAP + (blk + 1) * P, :], reads=[b_bufd], writes=[b_xe])

        assert NBLK == 2
        load_xe(0)
        for e in range(NE):
            if e + 2 < NE:
                load_w(e + 2)
            if e + 1 < NE:
                load_xe(e + 1)
            i = e % NWS
            xT_, b_xT_ = xeT[e % 2]
            for blk in range(NBLK):
                xet, b_xe = xe[(e * NBLK + blk) % 4]
                pt, b_pt = pts.next()
                for j in range(8):
                    tr(pt[:, j * P:(j + 1) * P], xet[:, j * P:(j + 1) * P], identb[:], [b_xe, b_identb], b_pt, j == 0)
                tt("dve", xT_[:, :, blk * P:(blk + 1) * P], pt[:].rearrange("p (j t) -> p j t", j=8),
                   gT[:, 16:24].unsqueeze(2).to_broadcast([P, 8, P]), ALU.mult, [b_pt, b_gT],
                   **(dict(writes=[b_xT_]) if blk == 0 else dict(joins=[b_xT_])))
            MPB = 512 // CAP
            sgt, b_sg = sg[e % 2]
            ht, b_ht = hTt[e % 2]
            gb = []
            for m in range(4):
                if m % MPB == 0:
                    gb.append(pbs.next())
                pb, b_pb = gb[-1]
                for j in range(8):
                    mm(pb[:, (m % MPB) * CAP:(m % MPB + 1) * CAP], wgv[i][:, j, m * P:(m + 1) * P], xT_[:, j, :], j == 0, j == 7,
                       [b_ws[i], b_xT_], b_pb, newgrp=(m % MPB == 0 and j == 0))
                if m % MPB == MPB - 1:
                    m0 = m - MPB + 1
                    act(sgt[:, m0:m + 1, :], pb[:, :].rearrange("p (a t) -> p a t", a=MPB), AF.Silu, [b_pb],
                        **(dict(writes=[b_sg]) if m0 == 0 else dict(joins=[b_sg])))
            for m in range(4):
                if m % MPB == 0:
                    gb.append(pbs.next())
                pb, b_pb = gb[-1]
                for j in range(8):
                    mm(pb[:, (m % MPB) * CAP:(m % MPB + 1) * CAP], wuv[i][:, j, m * P:(m + 1) * P], xT_[:, j, :], j == 0, j == 7,
                       [b_ws[i], b_xT_], b_pb, newgrp=(m % MPB == 0 and j == 0))
                if m % MPB == MPB - 1:
                    m0 = m - MPB + 1
                    tt("dve", ht[:, m0:m + 1, :], pb[:, :].rearrange("p (a t) -> p a t", a=MPB), sgt[:, m0:m + 1, :], ALU.mult,
                       [b_pb, b_sg], **(dict(writes=[b_ht]) if m0 == 0 else dict(joins=[b_ht])))
            for blk in range(NBLK):
                yot, b_yo = yo[ny % 4]
                for half in range(2):
                    pb, b_pb = pbs.next()
                    for m in range(4):
                        mm(pb[:, :], ht[:, m, blk * P:(blk + 1) * P], wdv[i][:, m, half * 512:(half + 1) * 512], m == 0, m == 3,
                           [b_ht, b_ws[i]], b_pb)
                    cp("act" if half == 0 else "dve", yot[:, half * 512:(half + 1) * 512], pb[:, :], [b_pb],
                       **(dict(writes=[b_yo]) if half == 0 else dict(joins=[b_yo])))
                p.dma("sp", dyo[ny % 4], ybuf_d[e * CAP + blk * P:e * CAP + (blk + 1) * P, :], yot[:], reads=[b_yo], joins=[b_ybufd])
                ny += 1

        p.barrier()
        aoff[0] = 0
        gfin, b_gfin = carve("gfin", [P, D], F32)
        dgf = p.dsem()
        p.dma("sp", dgf, gfin, gfin_d, writes=[b_gfin])
        junk, b_junk = carve("junk3", [P, D], BF16)
        r1 = slots("r1", 2, [P, D], F32)
        r2 = slots("r2", 2, [P, D], F32)
        h1r = slots("h1r", 2, [P, D], F32)
        ot_ = slots("ot", 2, [P, D], F32)
        st3 = slots("st3", 2, [P, 2], F32)
        dg = [[p.dsem() for _ in range(2)] for _ in range(2)]
        dhr = [p.dsem() for _ in range(2)]
        dot = [p.dsem() for _ in range(2)]
        finals = []
        for c in range(NCH):
            s = c % 2
            for k, (rr, b_rr) in enumerate((r1[s], r2[s])):
                p.dma_fn("pool", dg[s][k], lambda q, rr=rr, k=k, c=c: q.indirect_dma_start(
                    out=rr[:, :], out_offset=None, in_=ybuf_d[0:NSLOT, :],
                    in_offset=bass.IndirectOffsetOnAxis(ap=dest_all[:, 2 * c + k:2 * c + k + 1], axis=0)),
                    reads=[b_ybufd, b_dest], writes=[b_rr])
            hr, b_hr = h1r[s]
            if c == 0:
                p.dma("sp", dhr[s], hr[:], h1_d[c * P:(c + 1) * P, :], reads=[b_h1d], writes=[b_hr])
            if c + 1 < NCH:
                hrn, b_hrn = h1r[(c + 1) % 2]
                p.dma("sp", dhr[(c + 1) % 2], hrn[:], h1_d[(c + 1) * P:(c + 2) * P, :], reads=[b_h1d], writes=[b_hrn])
            stt(hr[:], r1[s][0][:], gate_all[:, 2 * c:2 * c + 1], hr[:], ALU.mult, ALU.add, [r1[s][1], b_gate, b_hr], writes=[b_hr])
            stt(hr[:], r2[s][0][:], gate_all[:, 2 * c + 1:2 * c + 2], hr[:], ALU.mult, ALU.add, [r2[s][1], b_gate, b_hr], writes=[b_hr])
            s3_, b_s3 = st3[s]
            act(junk[:], hr[:], AF.Square, [b_hr], writes=[b_junk, b_s3], accum_out=s3_[:, 0:1])
            rstd_from_ss(s3_[:, 0:1], s3_[:, 1:2], D, b_s3, b_s3)
            o_, b_o = ot_[s]
            stt(o_[:], hr[:], s3_[:, 1:2], gfin, ALU.mult, ALU.mult, [b_hr, b_s3, b_gfin], writes=[b_o])
            f = p.dma("sp", dot[s], out_d[c * P:(c + 1) * P, :], o_[:], reads=[b_o])
            finals.append(f)
        stats = p.emit(final_waits=finals[-2:])
    return nc, stats


def host_inputs(inp, S, NG, EPG, CAP, ncores):
    f = lambda a: np.ascontiguousarray(np.asarray(a, dtype=np.float32))
    w_in = f(inp["w_in"])[0]
    z, xbc, dt, q, k, v = np.split(w_in, [1024, 2560, 2576, 3600, 3728], axis=1)
    win = np.concatenate([z, xbc, q, k[:, 0:64], k[:, 0:64], k[:, 64:128], k[:, 64:128], v, dt], axis=1)
    assert win.shape[1] == NWIN
    cw = f(inp["conv_w"])[0]
    cwl = np.ascontiguousarray(cw.T.reshape(12, 128, 4).transpose(1, 0, 2).reshape(128, 48))
    sv = np.concatenate([np.broadcast_to(f(inp[n])[0][None, :], (128, 16)) for n in ("dt_bias", "a_log", "d_skip", "attn_sinks")], axis=1)
    tT = lambda v_: np.ascontiguousarray(f(v_).reshape(8, 128).T)
    gT = np.concatenate([tT(inp["ssd_norm"][0]), tT(inp["attn_norm"][0]), tT(inp["norm_ffn"][0]), tT(inp["norm_mix"][0])], axis=1)
    gfin = np.broadcast_to(f(inp["norm_final"])[None, :], (128, 1024))
    wr = np.concatenate([f(inp["w_router_group"])[0], f(inp["w_router_expert"])[0]], axis=1)
    NE = NG * EPG
    common = {
        "win": f(win), "wout": f(inp["w_out"])[0], "cw": f(cwl), "cb": f(inp["conv_b"])[0].reshape(1, 1536),
        "sv": f(sv), "gfin": f(gfin), "gT": f(gT), "wr": f(wr),
        "wg": f(inp["w_gate"])[0], "wu": f(inp["w_up"])[0], "wd": f(inp["w_down"])[0],
        "cst": make_consts(NE, CAP)[0], "cstb": make_consts(NE, CAP)[1],
    }
    x = f(inp["x"])
    maps = []
    for b in range(ncores):
        m = dict(common)
        m["x"] = np.ascontiguousarray(x[b])
        maps.append(m)
    return maps


_NC_CACHE = {}


def run_cfg(inp, S, NG, EPG, CAP, ncores, stop=0):
    key = (S, NG, EPG, CAP, stop)
    if key not in _NC_CACHE:
        _NC_CACHE[key] = build_nc(S, NG, EPG, CAP, stop)
    nc, stats = _NC_CACHE[key]
    maps = host_inputs(inp, S, NG, EPG, CAP, ncores)
    res = run_bass_kernel_spmd(nc, maps, core_ids=list(range(ncores)))
    return np.stack([np.asarray(r["out"], dtype=np.float32) for r in res.results], axis=0)


def kernel(**inputs):
    return run_cfg(inputs, 4096, 8, 8, 256, 8)
```

```python
import contextlib
import numpy as np
import ml_dtypes
import concourse.bass as bass
import concourse.mybir as mybir
from concourse.bass_utils import run_bass_kernel_spmd

F32 = mybir.dt.float32
BF16 = mybir.dt.bfloat16
I32 = mybir.dt.int32
AF = mybir.ActivationFunctionType
ALU = mybir.AluOpType
AX = mybir.AxisListType


class Buf:
    __slots__ = ("name", "w", "r", "war")

    def __init__(self, name):
        self.name = name
        self.w = []
        self.r = []
        self.war = []


class DSem:
    __slots__ = ("sem", "count", "name")

    def __init__(self, sem, name):
        self.sem = sem
        self.count = 0
        self.name = name


class Op:
    __slots__ = ("eng", "fn", "deps", "dsem", "dcount", "semval", "needed", "idx")


class Prog:
    ENGS = ("pe", "act", "dve", "pool", "sp")

    def __init__(self, nc, stack, strict_same=False):
        self.nc = nc
        self.stack = stack
        self.strict = strict_same
        self.ops = {e: [] for e in self.ENGS}
        self.esem = {e: stack.enter_context(nc.semaphore("es_" + e)) for e in self.ENGS}
        self.nds = 0
        self.dsems = []

    def dsem(self, name=None):
        self.nds += 1
        name = name or ("ds%d" % self.nds)
        d = DSem(self.stack.enter_context(self.nc.semaphore(name)), name)
        self.dsems.append(d)
        return d

    def barrier(self):
        marks = []
        for e in self.ENGS:
            m = self._record(e, lambda q: q.drain(fusable=False), (), (), (), None)
            m.needed = True
            marks.append(m)
        dm = []
        for d in self.dsems:
            if d.count > 0:
                f = Op()
                f.eng = None
                f.dsem = d
                f.dcount = d.count
                f.needed = False
                f.semval = None
                dm.append(f)
        for e in self.ENGS:
            o = self._record(e, lambda q: q.nop(), (), (), (), None)
            o.deps = [(m, 0) for m in marks if m.eng != e] + [(f, 0) for f in dm]

    def sb(self, name, shape, dt):
        return self.stack.enter_context(self.nc.sbuf_tensor("s_" + name, list(shape), dt))

    def ps(self, name, shape, dt):
        return self.stack.enter_context(self.nc.psum_tensor("p_" + name, list(shape), dt))

    def _record(self, eng, fn, reads, writes, joins, dsem):
        op = Op()
        op.eng = eng
        op.fn = fn
        op.dsem = dsem
        op.needed = False
        op.semval = None
        op.dcount = None
        if dsem is not None:
            dsem.count += 16
            op.dcount = dsem.count
        deps = []
        for b in reads:
            deps.extend((d, 0) for d in b.w)
        for b in writes:
            deps.extend((d, 1) for d in b.w)
            deps.extend((d, 2) for d in b.r)
        for b in joins:
            deps.extend((d, 2) for d in b.war)
            deps.extend((d, 2) for d in b.r)
        op.deps = deps
        for b in reads:
            b.r.append(op)
        for b in writes:
            b.w = [op]
            b.war = b.r
            b.r = []
        for b in joins:
            b.w.append(op)
            if b.r:
                b.war = b.war + b.r
            b.r = []
        op.idx = len(self.ops[eng])
        self.ops[eng].append(op)
        return op

    def op(self, eng, fn, reads=(), writes=(), joins=()):
        return self._record(eng, fn, reads, writes, joins, None)

    def dma(self, eng, dsem, out, in_, reads=(), writes=(), joins=(), **kw):
        return self._record(eng, lambda q: q.dma_start(out=out, in_=in_, **kw),
                            reads, writes, joins, dsem)

    def dma_fn(self, eng, dsem, fn, reads=(), writes=(), joins=()):
        return self._record(eng, fn, reads, writes, joins, dsem)

    def emit(self, final_waits=()):
        nc = self.nc
        for e in self.ENGS:
            for op in self.ops[e]:
                for d, kind in op.deps:
                    if d.dsem is None and (d.eng != e or (self.strict and e != "pe" and kind != 2)):
                        d.needed = True
        for d in final_waits:
            if d.dsem is None:
                d.needed = True
        for e in self.ENGS:
            c = 0
            for op in self.ops[e]:
                if op.dsem is None and op.needed:
                    c += 1
                    op.semval = c
        stats = {}

        def run(e, q):
            waited = {}
            nw = 0
            ops = self.ops[e]
            for op in ops:
                need = {}
                for d, kind in op.deps:
                    if d.dsem is not None:
                        key = ("d", id(d.dsem))
                        sem, val = d.dsem.sem, d.dcount
                    else:
                        if d.eng == e and (not self.strict or e == "pe" or kind == 2):
                            continue
                        key = ("e", d.eng)
                        sem, val = self.esem[d.eng], d.semval
                    if waited.get(key, 0) >= val:
                        continue
                    if key not in need or need[key][1] < val:
                        need[key] = (sem, val)
                for key, (sem, val) in need.items():
                    q.wait_ge(sem, val)
                    waited[key] = val
                    nw += 1
                ins = op.fn(q)
                if op.dsem is not None:
                    ins.then_inc(op.dsem.sem, 16)
                elif op.needed:
                    ins.then_inc(self.esem[e], 1)
            if e == "sp":
                for d in final_waits:
                    if d.dsem is not None:
                        q.wait_ge(d.dsem.sem, d.dcount)
                    else:
                        q.wait_ge(self.esem[d.eng], d.semval)
            stats[e] = (len(ops), nw)

        with nc.Block() as block:
            @block.tensor
            def _(q):
                run("pe", q)

            @block.scalar
            def _(q):
                run("act", q)

            @block.vector
            def _(q):
                run("dve", q)

            @block.gpsimd
            def _(q):
                run("pool", q)

            @block.sync
            def _(q):
                run("sp", q)
        return stats
D = 1024
NWIN = 3984
OZ, OX, OQ, OK_, OV, ODT = 0, 1024, 2560, 3584, 3840, 3968
NEG = -30000.0
BIG = 1.0e4
EPS = 1e-6


def make_consts(NE, CAP):
    i = np.arange(128)
    t = i[:, None]
    l = i[None, :]
    ident = (t == l)
    triinc = (t <= l)
    ones = np.ones((128, 128))
    U = (t > l)
    slt = (t < l)
    negprev = np.where(t > l, 0.0, NEG)
    negcur = np.where(t <= l, 0.0, NEG)
    ecap = np.broadcast_to((np.arange(NE) * CAP)[None, :], (128, NE))
    c32 = np.concatenate([ident, triinc, ones, U, ecap], axis=1)
    c16 = np.concatenate([ident, slt, ones] + [negprev] * 4 + [negcur] * 4 + [triinc], axis=1)
    return np.ascontiguousarray(c32.astype(np.float32)), np.ascontiguousarray(c16.astype(np.float32))


C_ID, C_TRI, C_ONE, C_U, C_EC = [k * 128 for k in range(5)]
NC16 = 3 * 128 + 8 * 128 + 128


def build_nc(S, NG, EPG, CAP, stop=0):
    NCH = S // 128
    NE = NG * EPG
    NR = NG + NE
    NSLOT = NE * CAP
    NBLK = CAP // 128
    CW = 4 * 128 + NE
    P = 128
    nc = bass.Bass("TRN2", target_bir_lowering=False)
    din = lambda n, s, dt=F32: nc.dram_tensor(n, list(s), dt, kind="ExternalInput").ap()
    x_d = din("x", [S, D])
    win_d = din("win", [D, NWIN])
    wout_d = din("wout", [2 * D, D])
    cw_d = din("cw", [P, 48])
    cb_d = din("cb", [1, 1536])
    sv_d = din("sv", [P, 64])
    gfin_d = din("gfin", [P, D])
    gT_d = din("gT", [P, 32])
    wr_d = din("wr", [D, NR])
    wg_d = din("wg", [NE, D, 512])
    wu_d = din("wu", [NE, D, 512])
    wd_d = din("wd", [NE, 512, D])
    cst_d = din("cst", [P, CW])
    cstb_d = din("cstb", [P, NC16])
    out_d = nc.dram_tensor("out", [S, D], F32, kind="ExternalOutput").ap()
    h1_d = nc.dram_tensor("h1s", [S, D], F32).ap()
    buf_d = nc.dram_tensor("bufs", [NSLOT + S, D], BF16).ap()
    ybuf_d = nc.dram_tensor("ybufs", [NSLOT + S, D], F32).ap()

    with contextlib.ExitStack() as st:
        p = Prog(nc, st, strict_same=True)
        sb = p.sb

        def T(name, shape, dt):
            return sb(name, shape, dt), Buf(name)

        UN = 8 * NWIN + 16 * D
        big = sb("big", [P, UN], BF16)
        b_big = Buf("big")
        win = big[:, 0:8 * NWIN].rearrange("p (j n) -> p j n", j=8)
        wout = big[:, 8 * NWIN:UN].rearrange("p (j n) -> p j n", j=16)
        cst, b_cst = T("cst", [P, CW], F32)
        cb16, b_cb16 = T("cb16", [P, NC16], BF16)
        identb = cb16[:, 0:128]
        sltb = cb16[:, 128:256]
        onesb = cb16[:, 256:384]
        negp = cb16[:, 384:896]
        negc = cb16[:, 896:1408]
        tri16 = cb16[:, 1408:1536]
        b_identb = b_sltb = b_onesb = b_negp = b_negc = b_cb16
        cwt, b_cwt = T("cwt", [P, 48], F32)
        cdiag, b_cdiag = T("cdiag", [P, 48, P], BF16)
        cbb, b_cbb = T("cbb", [1, 1536], BF16)
        sv, b_sv = T("sv", [P, 64], F32)
        aneg, b_aneg = T("aneg", [P, 16], F32)
        esink, b_esink = T("esink", [P, 16], F32)
        gT, b_gT = T("gT", [P, 32], F32)
        wr, b_wr = T("wr", [P, 8, NR], F32)
        epst, b_epst = T("epst", [P, 2], F32)
        S32, b_S32 = T("S32", [P, D], F32)
        Sbf, b_Sbf = T("Sbf", [P, D], BF16)
        mcum, b_mcum = T("mcum", [P, NE], BF16)
        dest_all, b_dest = T("dest_all", [P, NCH * 2], I32)
        gate_all, b_gate = T("gate_all", [P, NCH * 2], F32)
        xbcraw, b_xbcraw = T("xbcraw", [P, 12, 132], BF16)
        xbcrawB, b_xbcrawB = T("xbcrawB", [P, 12, 132], BF16)
        AW = 19820
        arena = sb("arena", [P, AW], F32)
        aoff = [0]

        def carve(name, shape, dt):
            n = 1
            for d_ in shape[1:]:
                n *= d_
            nb = n * (4 if dt in (F32, I32) else 2)
            nw = (nb + 3) // 4
            a = aoff[0]
            aoff[0] += nw
            assert aoff[0] <= AW, ("arena overflow", name, aoff[0])
            v = arena[:, a:a + nw]
            if dt != F32:
                v = v.bitcast(dt)
            v = v[:, 0:n]
            if len(shape) == 3:
                v = v.rearrange("p (a b) -> p a b", a=shape[1])
            return v, Buf(name)

        class Rot:
            def __init__(self, tiles):
                self.t = tiles
                self.i = 0

            def next(self):
                r = self.t[self.i]
                self.i = (self.i + 1) % len(self.t)
                return r

        pbs = Rot([(p.ps("pb%d" % i, [P, 512], F32), Buf("pb%d" % i)) for i in range(6)])
        pts = Rot([(p.ps("pt%d" % i, [P, 1024], BF16), Buf("pt%d" % i)) for i in range(2)])

        def mm(out, lhsT, rhs, first, last, reads, bank, newgrp=None):
            new = first if newgrp is None else newgrp
            kw = dict(writes=[bank]) if new else dict(joins=[bank])
            p.op("pe", lambda q: q.matmul(out=out, lhsT=lhsT, rhs=rhs, start=first, stop=last), reads=reads, **kw)

        def tr(out, in_, ident, reads, bank, new):
            kw = dict(writes=[bank]) if new else dict(joins=[bank])
            p.op("pe", lambda q: q.transpose(out=out, in_=in_, identity=ident), reads=reads, **kw)

        def act(out, in_, func, reads, writes=(), joins=(), **kw):
            p.op("act", lambda q: q.activation(out=out, in_=in_, func=func, **kw), reads=reads, writes=writes, joins=joins)

        def tt(eng, out, in0, in1, op, reads, writes=(), joins=()):
            p.op(eng, lambda q: q.tensor_tensor(out=out, in0=in0, in1=in1, op=op), reads=reads, writes=writes, joins=joins)

        def ts(eng, out, in0, s1, op0, reads, writes=(), joins=(), s2=None, op1=None):
            if op1 is None:
                p.op(eng, lambda q: q.tensor_scalar(out=out, in0=in0, scalar1=s1, scalar2=None, op0=op0),
                     reads=reads, writes=writes, joins=joins)
            else:
                p.op(eng, lambda q: q.tensor_scalar(out=out, in0=in0, scalar1=s1, scalar2=s2, op0=op0, op1=op1),
                     reads=reads, writes=writes, joins=joins)

        def stt(out, in0, scalar, in1, op0, op1, reads, writes=(), joins=()):
            p.op("dve", lambda q: q.scalar_tensor_tensor(out=out, in0=in0, scalar=scalar, in1=in1, op0=op0, op1=op1),
                 reads=reads, writes=writes, joins=joins)

        def cp(eng, out, in_, reads, writes=(), joins=()):
            if eng == "act":
                act(out, in_, AF.Copy, reads, writes, joins)
            else:
                p.op(eng, lambda q: q.tensor_copy(out=out, in_=in_), reads=reads, writes=writes, joins=joins)

        def rstd_from_ss(ss_ap, rs_ap, n, b_ss, b_rs):
            act(rs_ap, ss_ap, AF.Ln, [b_ss, b_epst], writes=[b_rs], bias=epst[:, 0:1], scale=1.0 / n)
            act(rs_ap, rs_ap, AF.Exp, [b_rs], writes=[b_rs], scale=-0.5)

        dI = [p.dsem() for _ in range(12)]
        p.dma("sp", dI[0], cst[:], cst_d, writes=[b_cst])
        p.dma("sp", dI[1], cwt[:], cw_d, writes=[b_cwt])
        p.dma("pool", dI[2], cbb[:], cb_d, writes=[b_cbb])
        p.dma("sp", dI[3], sv[:], sv_d, writes=[b_sv])
        p.dma("sp", dI[5], gT[:], gT_d, writes=[b_gT])
        p.dma("sp", dI[6], wr[:], wr_d.rearrange("(j p) n -> p j n", p=P), writes=[b_wr])
        b_wout = Buf("wout")
        for j in range(8):
            p.dma("pool", dI[7], win[:, j, :], win_d[j * P:(j + 1) * P, :], **(dict(writes=[b_big]) if j == 0 else dict(joins=[b_big])))
        for j in range(16):
            p.dma("pool", dI[9], wout[:, j, :], wout_d[j * P:(j + 1) * P, :], **(dict(writes=[b_wout]) if j == 0 else dict(joins=[b_wout])))
        p.op("pool", lambda q: q.memset(epst[:, 0:1], EPS), writes=[b_epst])
        p.op("pool", lambda q: q.memset(epst[:, 1:2], 1.0), joins=[b_epst])
        p.op("pool", lambda q: q.memset(S32[:], 0.0), writes=[b_S32])
        p.op("pool", lambda q: q.memset(Sbf[:], 0.0), writes=[b_Sbf])
        p.op("pool", lambda q: q.memset(mcum[:], 0.0), writes=[b_mcum])
        p.op("pool", lambda q: q.memset(xbcraw[:], 0.0), writes=[b_xbcraw])
        p.op("pool", lambda q: q.memset(xbcrawB[:], 0.0), writes=[b_xbcrawB])
        p.dma("pool", dI[8], cb16[:], cstb_d, writes=[b_cb16])
        tt("pool", cdiag[:], identb.unsqueeze(1).to_broadcast([P, 48, P]),
           cwt[:].unsqueeze(2).to_broadcast([P, 48, P]), ALU.mult, [b_identb, b_cwt], writes=[b_cdiag])
        act(aneg[:], sv[:, 16:32], AF.Exp, [b_sv], writes=[b_aneg])
        ts("pool", aneg[:], aneg[:], -1.0, ALU.mult, [b_aneg], writes=[b_aneg])
        act(esink[:], sv[:, 48:64], AF.Exp, [b_sv], writes=[b_esink])
        ident32 = cst[:, C_ID:C_ID + P]
        tri32 = cst[:, C_TRI:C_TRI + P]
        ones32 = cst[:, C_ONE:C_ONE + P]
        U32 = cst[:, C_U:C_U + P]

        def slots(name, n, shape, dt):
            return [carve("%s%d" % (name, i), shape, dt) for i in range(n)]

        xc = slots("xc", 1, [P, D], F32) * 2
        dx = [p.dsem()] * 2
        hTb = slots("hTb", 1, [P, 8, P], F32)
        dxo = p.dsem()
        stF = slots("stF", 2, [P, 2], F32)
        stS = slots("stS", 1, [P, 4], F32)
        stA = slots("stA", 1, [P, 2], F32)
        stT = slots("stT", 1, [P, 2], F32)
        aden = slots("aden", 1, [P, 16], F32)
        xn = slots("xn", 1, [P, D], BF16)
        junk, b_junk = carve("junk", [P, D], BF16)
        xnT = slots("xnT", 1, [P, 8, P], BF16)
        sz = slots("sz", 2, [P, D], BF16)
        xbcT = slots("xbcT", 2, [P, 12, P], BF16)
        qT = slots("qT", 2, [P, 8, P], BF16)
        kT = slots("kT", 3, [P, 2, P], BF16)
        Vp = slots("Vp", 3, [P, 2, 66], BF16)
        dts = slots("dts", 2, [P, 16 * 8], F32)
        xs_tok = slots("xs_tok", 1, [P, D], BF16)
        B_tok = slots("B_tok", 1, [P, 2 * P], BF16)
        xdt = slots("xdt", 1, [P, D], BF16)
        xdt2 = slots("xdt2", 1, [P, D], BF16)
        segLh = slots("segLh", 1, [P, 8, P], BF16)
        segLl = slots("segLl", 1, [P, 8, P], BF16)
        ahl = slots("ahl", 1, [P, 32], BF16)
        alo32 = slots("alo32", 1, [P, 16], F32)
        dec = slots("dec", 2, [P, 4, P], BF16)
        MT = slots("MT", 1, [P, 16, P], BF16)
        cbm = slots("cbm", 1, [P, 2, P], BF16)
        ya = slots("ya", 1, [P, D], F32)
        uu = slots("uu", 1, [P, D], F32)
        PT = [[carve("PT%d_%d" % (i, k), [P, 4, P], BF16) for k in range(2)] for i in range(2)]
        attraw = slots("attraw", 1, [P, 16, 65], F32)
        mixed = slots("mixed", 1, [P, 2 * D], BF16)
        mixT = slots("mixT", 1, [P, 16, P], BF16)
        dh1 = [p.dsem() for _ in range(2)]
        hnb = slots("hnb", 1, [P, D], BF16)
        dsc = [p.dsem() for _ in range(2)]
        rt = slots("rt", 1, [P, NR + 16 + 3 * NG + 7 * NE], F32)
        mselb = slots("mselb", 1, [P, NE], BF16)
        b_h1d = Buf("h1_d")
        b_bufd = Buf("buf_d")
        b_bufz = Buf("buf_z")
        b_ybufd = Buf("ybuf_d")
        for i in range(3):
            p.op("pool", lambda q, i=i: q.memset(Vp[i][0][:], 1.0), writes=[Vp[i][1]])

        def wj(first, b):
            return dict(writes=[b]) if first else dict(joins=[b])

        _mxseen = set()

        def mxfirst(c):
            if c in _mxseen:
                return False
            _mxseen.add(c)
            return True

        def front_dma(c):
            if c >= NCH:
                return
            xct, b_xc = xc[c % 2]
            p.dma("sp", dx[c % 2], xct[:], x_d[c * P:(c + 1) * P, :], writes=[b_xc])

        def front_pro(c):
            if c >= NCH:
                return
            s2 = c % 2
            xct, b_xc = xc[s2]
            stt_, b_st = stF[s2]
            act(junk[:], xct[:], AF.Square, [b_xc], writes=[b_st, b_junk], accum_out=stt_[:, 0:1])
            rstd_from_ss(stt_[:, 0:1], stt_[:, 1:2], D, b_st, b_st)
            xnt, b_xn = xn[0]
            act(xnt[:], xct[:], AF.Copy, [b_xc, b_st], writes=[b_xn], scale=stt_[:, 1:2])

        def front(c):
            s2, s3 = c % 2, c % 3
            xnt, b_xn = xn[0]
            pt, b_pt = pts.next()
            for j in range(8):
                tr(pt[:, j * P:(j + 1) * P], xnt[:, j * P:(j + 1) * P], identb, [b_xn, b_cb16], b_pt, j == 0)
            xT, b_xT = xnT[0]
            for j in range(8):
                act(xT[:, j, :], pt[:, j * P:(j + 1) * P], AF.Copy, [b_pt, b_gT], scale=gT[:, 24 + j:25 + j], **wj(j == 0, b_xT))
            yield
            kt_, b_kt = kT[s3]
            vp, b_vp = Vp[s3]
            dt_, b_dt = dts[s2]
            pb, b_pb = pbs.next()
            for g in range(2):
                for j in range(8):
                    mm(pb[:, g * P:(g + 1) * P], win[:, j, OK_ + g * P:OK_ + (g + 1) * P], xT[:, j, :],
                       j == 0, j == 7, [b_xT, b_big], b_pb, newgrp=(g == 0 and j == 0))
            for j in range(8):
                mm(pb[:, 256:384], xT[:, j, :], win[:, j, OV:OV + 128], j == 0, j == 7, [b_xT, b_big], b_pb, newgrp=False)
            for j in range(8):
                mm(pb[:, 384:400], xT[:, j, :], win[:, j, ODT:ODT + 16], j == 0, False, [b_xT, b_big], b_pb, newgrp=False)
            mm(pb[:, 384:400], ones32[0:1, :], sv[0:1, 0:16], False, True, [b_cst, b_sv], b_pb, newgrp=False)
            cp("act", kt_[:], pb[:, 0:256].rearrange("p (g t) -> p g t", g=2), [b_pb], writes=[b_kt])
            cp("act", vp[:, :, 0:64], pb[:, 256:384].rearrange("p (g d) -> p g d", g=2), [b_pb], writes=[b_vp])
            act(dt_[:, 0:16], pb[:, 384:400], AF.Exp, [b_pb], writes=[b_dt])
            act(dt_[:, 16:32], dt_[:, 0:16], AF.Ln, [b_dt, b_epst], writes=[b_dt], bias=epst[:, 1:2])
            tt("dve", dt_[:, 32:48], dt_[:, 16:32], aneg[:], ALU.mult, [b_dt, b_aneg], writes=[b_dt])
            front_dma(c + 1)
            yield
            szt, b_sz = sz[s2]
            for half in range(2):
                pb, b_pb = pbs.next()
                for j in range(8):
                    mm(pb[:, :], xT[:, j, :], win[:, j, OZ + half * 512:OZ + (half + 1) * 512], j == 0, j == 7, [b_xT, b_big], b_pb)
                act(szt[:, half * 512:(half + 1) * 512], pb[:, :], AF.Silu, [b_pb], **wj(half == 0, b_sz))
            yield
            for grp in range(3):
                pb, b_pb = pbs.next()
                for mm_ in range(4):
                    m = grp * 4 + mm_
                    for j in range(8):
                        mm(pb[:, mm_ * P:(mm_ + 1) * P], win[:, j, OX + m * P:OX + (m + 1) * P], xT[:, j, :],
                           j == 0, j == 7, [b_xT, b_big], b_pb, newgrp=(mm_ == 0 and j == 0))
                cp("act", xbcraw[:, grp * 4:(grp + 1) * 4, 4:132], pb[:, :].rearrange("p (a t) -> p a t", a=4), [b_pb],
                   **wj(grp == 0, b_xbcraw))
            yield
            p.dma("sp", dxo, xbcrawB[:, :, 0:131], xbcraw[:, :, 1:132], reads=[b_xbcraw], writes=[b_xbcrawB])
            xbt, b_xbt = xbcT[s2]
            for grp in range(3):
                pb, b_pb = pbs.next()
                for mm_ in range(4):
                    m = grp * 4 + mm_
                    for k in range(4):
                        src_, b_src = (xbcraw, b_xbcraw) if k % 2 == 1 else (xbcrawB, b_xbcrawB)
                        o_ = k + 1 if k % 2 == 1 else k
                        mm(pb[:, mm_ * P:(mm_ + 1) * P], cdiag[:, m * 4 + k, :], src_[:, m, o_:o_ + P],
                           k == 0, False, [b_cdiag, b_src], b_pb, newgrp=(mm_ == 0 and k == 0))
                    mm(pb[:, mm_ * P:(mm_ + 1) * P], cbb[0:1, m * P:(m + 1) * P], onesb[0:1, :], False, True,
                       [b_cbb, b_cb16], b_pb, newgrp=False)
                act(xbt[:, grp * 4:(grp + 1) * 4, :], pb[:, :].rearrange("p (a t) -> p a t", a=4), AF.Silu, [b_pb],
                    **wj(grp == 0, b_xbt))
                if grp == 2:
                    cp("pool", xbcraw[:, :, 0:4], xbcraw[:, :, 128:132], [b_xbcraw], writes=[b_xbcraw])
            front_pro(c + 1)
            yield
            qt, b_qt = qT[s2]
            for grp in range(2):
                pb, b_pb = pbs.next()
                for mm_ in range(4):
                    m = grp * 4 + mm_
                    for j in range(8):
                        mm(pb[:, mm_ * P:(mm_ + 1) * P], win[:, j, OQ + m * P:OQ + (m + 1) * P], xT[:, j, :],
                           j == 0, j == 7, [b_xT, b_big], b_pb, newgrp=(mm_ == 0 and j == 0))
                cp("dve", qt[:, grp * 4:(grp + 1) * 4, :], pb[:, :].rearrange("p (a t) -> p a t", a=4), [b_pb],
                   **wj(grp == 0, b_qt))
            yield
        def ssd(c):
            s2 = c % 2
            stt_, b_st = stS[0]
            xbt, b_xbt = xbcT[s2]
            szt, b_sz = sz[s2]
            dt_, b_dt = dts[s2]
            dtv = dt_[:, 16:32]
            av = dt_[:, 32:48]
            ev = dt_[:, 96:112]
            cdv = dt_[:, 112:128]
            xst, b_xst = xs_tok[0]
            Bt, b_Bt = B_tok[0]
            xd, b_xd = xdt[0]
            xd2, b_xd2 = xdt2[0]
            cb_, b_cb = cbm[0]
            sLh, b_sL = segLh[0]
            sLl = segLl[0][0]
            ah, b_ah = ahl[0]
            al32, b_al = alo32[0]
            mt, b_mt = MT[0]
            yat, b_ya = ya[0]
            ut, b_u = uu[0]
            mx, b_mx = mixed[0]

            def seg_heads(hq):
                pb, b_pb = pbs.next()
                for hh in range(4):
                    i_ = (hq % 2) * 4 + hh
                    mm(pb[:, hh * P:(hh + 1) * P], sLh[:, i_, :], tri16, True, False, [b_sL, b_cb16], b_pb, newgrp=(hh == 0))
                    mm(pb[:, hh * P:(hh + 1) * P], sLl[:, i_, :], tri16, False, True, [b_sL, b_cb16], b_pb, newgrp=False)
                dc, b_dc = dec[hq % 2]
                act(dc[:], pb[:, :].rearrange("p (a t) -> p a t", a=4), AF.Exp, [b_pb], writes=[b_dc])
                tt("dve", mt[:, hq * 4:(hq + 1) * 4, :], dc[:],
                   cb_[:, hq // 2, :].unsqueeze(1).to_broadcast([P, 4, P]), ALU.mult, [b_dc, b_cb], **wj(hq == 0, b_mt))

            def seg_build(h0):
                tt("pool", sLh[:], U32.unsqueeze(1).to_broadcast([P, 8, P]),
                   ah[:, h0:h0 + 8].unsqueeze(2).to_broadcast([P, 8, P]), ALU.mult, [b_cst, b_ah], writes=[b_sL])
                tt("pool", sLl[:], U32.unsqueeze(1).to_broadcast([P, 8, P]),
                   ah[:, 16 + h0:16 + h0 + 8].unsqueeze(2).to_broadcast([P, 8, P]), ALU.mult, [b_cst, b_ah], joins=[b_sL])

            cp("dve", ah[:, 0:16], av, [b_dt], writes=[b_ah])
            tt("dve", al32[:], av, ah[:, 0:16], ALU.subtract, [b_dt, b_ah], writes=[b_al])
            cp("dve", ah[:, 16:32], al32[:], [b_al], joins=[b_ah])
            seg_build(0)
            pb, b_pb = pbs.next()
            mm(pb[:, 0:16], tri32, av, True, True, [b_cst, b_dt], b_pb)
            mm(pb[:, 16:32], ones32, av, True, True, [b_cst, b_dt], b_pb, newgrp=False)
            cp("act", dt_[:, 48:80], pb[:, 0:32], [b_pb], writes=[b_dt])
            pt, b_pt = pts.next()
            for j in range(8):
                tr(pt[:, j * P:(j + 1) * P], xbt[:, j, :], identb, [b_xbt, b_cb16], b_pt, j == 0)
            cp("act", xst[:], pt[:], [b_pt], writes=[b_xst])
            pt, b_pt = pts.next()
            for g in range(2):
                tr(pt[:, g * P:(g + 1) * P], xbt[:, 8 + g, :], identb, [b_xbt, b_cb16], b_pt, g == 0)
            cp("dve", Bt[:], pt[:, 0:2 * P], [b_pt], writes=[b_Bt])
            pb, b_pb = pbs.next()
            for g in range(2):
                mm(pb[:, g * P:(g + 1) * P], xbt[:, 8 + g, :], xbt[:, 10 + g, :], True, True, [b_xbt], b_pb, newgrp=(g == 0))
            tt("dve", cb_[:], pb[:, 0:256].rearrange("p (g t) -> p g t", g=2), tri32.unsqueeze(1).to_broadcast([P, 2, P]),
               ALU.mult, [b_pb, b_cst], writes=[b_cb])
            tt("pool", dt_[:, 80:96], dt_[:, 64:80], dt_[:, 48:64], ALU.subtract, [b_dt], writes=[b_dt])
            act(dt_[:, 96:112], dt_[:, 48:64], AF.Exp, [b_dt], writes=[b_dt])
            act(dt_[:, 80:96], dt_[:, 80:96], AF.Exp, [b_dt], writes=[b_dt])
            act(dt_[:, 112:128], dt_[:, 64:80], AF.Exp, [b_dt], writes=[b_dt])
            tt("pool", dt_[:, 0:16], dtv, dt_[:, 80:96], ALU.mult, [b_dt], writes=[b_dt])
            tt("dve", xd[:].rearrange("p (h d) -> p h d", h=16), xst[:].rearrange("p (h d) -> p h d", h=16),
               dtv.unsqueeze(2).to_broadcast([P, 16, 64]), ALU.mult, [b_xst, b_dt], writes=[b_xd])
            tt("pool", xd2[:].rearrange("p (h d) -> p h d", h=16), xst[:].rearrange("p (h d) -> p h d", h=16),
               dt_[:, 0:16].unsqueeze(2).to_broadcast([P, 16, 64]), ALU.mult, [b_xst, b_dt], writes=[b_xd2])
            yield
            seg_heads(0)
            seg_heads(1)
            seg_build(8)
            pbo = [pbs.next() for _ in range(2)]
            for g in range(2):
                mm(pbo[g][0][:, :], xbt[:, 10 + g, :], Sbf[:, g * 512:(g + 1) * 512], True, True, [b_xbt, b_Sbf], pbo[g][1])
            for g in range(2):
                tt("dve", yat[:, g * 512:(g + 1) * 512].rearrange("p (h d) -> p h d", h=8),
                   pbo[g][0][:, :].rearrange("p (h d) -> p h d", h=8),
                   ev[:, g * 8:(g + 1) * 8].unsqueeze(2).to_broadcast([P, 8, 64]), ALU.mult, [pbo[g][1], b_dt], **wj(g == 0, b_ya))
            yield
            seg_heads(2)
            seg_heads(3)
            pbn = [pbs.next() for _ in range(2)]
            for g in range(2):
                mm(pbn[g][0][:, :], Bt[:, g * P:(g + 1) * P], xd2[:, g * 512:(g + 1) * 512], True, True, [b_Bt, b_xd2], pbn[g][1])
            tt("pool", S32[:].rearrange("p (h d) -> p h d", h=16), S32[:].rearrange("p (h d) -> p h d", h=16),
               cdv.unsqueeze(2).to_broadcast([P, 16, 64]), ALU.mult, [b_S32, b_dt], writes=[b_S32])
            for g in range(2):
                tt("dve", S32[:, g * 512:(g + 1) * 512], S32[:, g * 512:(g + 1) * 512], pbn[g][0][:, :], ALU.add,
                   [b_S32, pbn[g][1]], **wj(g == 0, b_S32))
            cp("pool", Sbf[:], S32[:], [b_S32], writes=[b_Sbf])
            yield
            pbd = [pbs.next() for _ in range(2)]
            for h in range(16):
                g = h // 8
                mm(pbd[g][0][:, (h % 8) * 64:(h % 8 + 1) * 64], mt[:, h, :], xd[:, h * 64:(h + 1) * 64], True, True,
                   [b_mt, b_xd], pbd[g][1], newgrp=(h % 8 == 0))
            for g in range(2):
                tt("dve", yat[:, g * 512:(g + 1) * 512], yat[:, g * 512:(g + 1) * 512], pbd[g][0][:, :], ALU.add,
                   [b_ya, pbd[g][1]], **wj(g == 0, b_ya))
            tt("pool", ut[:].rearrange("p (h d) -> p h d", h=16), xst[:].rearrange("p (h d) -> p h d", h=16),
               sv[:, 32:48].unsqueeze(2).to_broadcast([P, 16, 64]), ALU.mult, [b_xst, b_sv], writes=[b_u])
            tt("pool", yat[:], yat[:], ut[:], ALU.add, [b_u, b_ya], writes=[b_ya])
            tt("pool", ut[:], yat[:], szt[:], ALU.mult, [b_ya, b_sz], writes=[b_u])
            yield
            for g in range(2):
                act(junk[:, 0:512], ut[:, g * 512:(g + 1) * 512], AF.Square, [b_u], writes=[b_st, b_junk],
                    accum_out=stt_[:, g:g + 1])
            rstd_from_ss(stt_[:, 0:2], stt_[:, 2:4], 512, b_st, b_st)
            for g in range(2):
                ts("dve", mx[:, g * 512:(g + 1) * 512], ut[:, g * 512:(g + 1) * 512], stt_[:, 2 + g:3 + g], ALU.mult,
                   [b_u, b_st], **wj(mxfirst(c), b_mx))
            yield

        def attn(c):
            s2, s3 = c % 2, c % 3
            stt_, b_st = stA[0]
            qt, b_qt = qT[s2]
            ar, b_ar = attraw[0]
            first_ar = True
            kts = ([(c - 1) % 3] if c > 0 else []) + [s3]
            for g in range(2):
                for par in range(2):
                    for ki, ks in enumerate(kts):
                        isprev = (c > 0 and ki == 0)
                        pb, b_pb = pbs.next()
                        ktt, b_ktt = kT[ks]
                        mm(pb[:, :], ktt[par * 64:(par + 1) * 64, g, :], qt[par * 64:(par + 1) * 64, g * 4:(g + 1) * 4, :],
                           True, False, [b_ktt, b_qt], b_pb)
                        mm(pb[:, :], identb, negp if isprev else negc, False, True, [b_cb16], b_pb, newgrp=False)
                        ptile, b_ptile = PT[par][0 if isprev else 1]
                        act(ptile[:], pb[:, :].rearrange("p (a t) -> p a t", a=4), AF.Exp, [b_pb], writes=[b_ptile], scale=0.125)
                    pb, b_pb = pbs.next()
                    for t4 in range(4):
                        for ki, ks in enumerate(kts):
                            isprev = (c > 0 and ki == 0)
                            ptile, b_ptile = PT[par][0 if isprev else 1]
                            vpt, b_vpt = Vp[ks]
                            mm(pb[:, t4 * 65:(t4 + 1) * 65], ptile[:, t4, :], vpt[:, g, 0:65], ki == 0, ki == len(kts) - 1,
                               [b_ptile, b_vpt], b_pb, newgrp=(t4 == 0 and ki == 0))
                    cp("act", ar[:, g * 8 + par:g * 8 + 8:2, :], pb[:, 0:260].rearrange("p (a t) -> p a t", a=4), [b_pb],
                       **wj(first_ar, b_ar))
                    first_ar = False
                    yield
            dn, b_dn = aden[0]
            tt("dve", dn[:], ar[:, :, 64], esink[:], ALU.add, [b_ar, b_esink], writes=[b_dn])
            p.op("dve", lambda q: q.reciprocal(out=dn[:], in_=dn[:]), reads=[b_dn], writes=[b_dn])
            at3 = ar[:, :, 0:64]
            tt("dve", at3, at3, dn[:].unsqueeze(2).to_broadcast([P, 16, 64]), ALU.mult, [b_ar, b_dn], writes=[b_ar])
            act(junk[:].rearrange("p (h d) -> p h d", h=16), at3, AF.Square, [b_ar], writes=[b_st, b_junk], accum_out=stt_[:, 0:1])
            rstd_from_ss(stt_[:, 0:1], stt_[:, 1:2], D, b_st, b_st)
            mx, b_mx = mixed[0]
            ts("dve", mx[:, D:2 * D].rearrange("p (h d) -> p h d", h=16), at3, stt_[:, 1:2], ALU.mult, [b_ar, b_st], **wj(mxfirst(c), b_mx))
            yield

        dxr = p.dsem()

        def tail(c):
            s2 = c % 2
            mx, b_mx = mixed[0]
            mxT, b_mxT = mixT[0]
            ut, b_u = uu[0]
            p.dma("sp", dxr, ut[:], x_d[c * P:(c + 1) * P, :], writes=[b_u])
            for hb in range(2):
                pt, b_pt = pts.next()
                for j in range(8):
                    tr(pt[:, j * P:(j + 1) * P], mx[:, (hb * 8 + j) * P:(hb * 8 + j + 1) * P], identb, [b_mx, b_cb16], b_pt, j == 0)
                tt("dve", mxT[:, hb * 8:(hb + 1) * 8, :], pt[:].rearrange("p (j t) -> p j t", j=8),
                   gT[:, hb * 8:(hb + 1) * 8].unsqueeze(2).to_broadcast([P, 8, P]), ALU.mult, [b_pt, b_gT], **wj(hb == 0, b_mxT))
            if (c - 1) in pending:
                pending.pop(c - 1)()
            yield
            for half in range(2):
                pb, b_pb = pbs.next()
                for j in range(16):
                    mm(pb[:, :], mxT[:, j, :], wout[:, j, half * 512:(half + 1) * 512], j == 0, j == 15, [b_mxT, b_wout], b_pb)
                tt("dve", ut[:, half * 512:(half + 1) * 512], pb[:, :], ut[:, half * 512:(half + 1) * 512], ALU.add,
                   [b_pb, b_u], writes=[b_u])
                if half == 1:
                    f_ = p.dma("sp", dh1[s2], (out_d if stop else h1_d)[c * P:(c + 1) * P, :], ut[:], reads=[b_u], joins=[b_h1d])
                    finals1.append(f_)
            yield

        pending = {}

        def trt(c):
            yield from tail(c)
            if not stop:
                yield from rtail(c)

        def rtail(c):
            stt_, b_st = stT[0]
            ut, b_u = uu[0]
            act(junk[:], ut[:], AF.Square, [b_u], writes=[b_st, b_junk], accum_out=stt_[:, 0:1])
            rstd_from_ss(stt_[:, 0:1], stt_[:, 1:2], D, b_st, b_st)
            ts("dve", ut[:], ut[:], stt_[:, 1:2], ALU.mult, [b_u, b_st], writes=[b_u])
            hb_, b_hb = hnb[0]
            cp("act", hb_[:], ut[:], [b_u], writes=[b_hb])
            hT, b_hT = hTb[0]
            for hf in range(2):
                pb, b_pb = pbs.next()
                for j in range(4):
                    tr(pb[:, j * P:(j + 1) * P], ut[:, (hf * 4 + j) * P:(hf * 4 + j + 1) * P], ident32, [b_u, b_cst], b_pb, j == 0)
                tt("dve", hT[:, hf * 4:(hf + 1) * 4, :], pb[:, :].rearrange("p (j t) -> p j t", j=4),
                   gT[:, 16 + hf * 4:16 + (hf + 1) * 4].unsqueeze(2).to_broadcast([P, 4, P]), ALU.mult, [b_pb, b_gT],
                   **wj(hf == 0, b_hT))
            yield
            pb, b_pb = pbs.next()
            for j in range(8):
                mm(pb[:, 0:NR], hT[:, j, :], wr[:, j, :], j == 0, j == 7, [b_hT, b_wr], b_pb)
            r, b_r = rt[0]
            o = [0]

            def seg(n):
                a = r[:, o[0]:o[0] + n]
                o[0] += n
                return a

            lg = seg(NR)
            gmax, ngmax, gsum, pgrp, m1, m2, dd, ex, den, g1, g2, d1, d2 = [seg(1) for _ in range(13)]
            goh, pen, gex = seg(NG), seg(NG), seg(NG)
            ml, oh1, ml2, oh2, msel, rk, tmp = [seg(NE) for _ in range(7)]
            cp("act", lg, pb[:, 0:NR], [b_pb], writes=[b_r])
            yield
            R = [b_r]
            W = dict(writes=[b_r])
            p.op("dve", lambda q: q.tensor_reduce(out=gmax, in_=lg[:, 0:NG], axis=AX.X, op=ALU.max), reads=R, **W)
            ts("dve", goh, lg[:, 0:NG], gmax, ALU.is_equal, R, **W)
            ts("dve", ngmax, gmax, -1.0, ALU.mult, R, **W)
            act(gex, lg[:, 0:NG], AF.Exp, R, bias=ngmax, accum_out=gsum, **W)
            p.op("dve", lambda q: q.reciprocal(out=pgrp, in_=gsum), reads=R, **W)
            ts("dve", pen, goh, -1.0, ALU.add, R, s2=BIG, op1=ALU.mult, **W)
            tt("dve", ml.rearrange("p (g e) -> p g e", g=NG), lg[:, NG:NR].rearrange("p (g e) -> p g e", g=NG),
               pen.unsqueeze(2).to_broadcast([P, NG, EPG]), ALU.add, R, **W)
            p.op("dve", lambda q: q.tensor_reduce(out=m1, in_=ml, axis=AX.X, op=ALU.max), reads=R, **W)
            ts("dve", oh1, ml, m1, ALU.is_equal, R, **W)
            stt(ml2, oh1, -BIG, ml, ALU.mult, ALU.add, R, **W)
            p.op("dve", lambda q: q.tensor_reduce(out=m2, in_=ml2, axis=AX.X, op=ALU.max), reads=R, **W)
            ts("dve", oh2, ml2, m2, ALU.is_equal, R, **W)
            yield
            tt("dve", dd, m2, m1, ALU.subtract, R, **W)
            act(ex, dd, AF.Exp, R, **W)
            ts("dve", den, ex, 1.0, ALU.add, R, **W)
            p.op("dve", lambda q: q.reciprocal(out=g1, in_=den), reads=R, **W)
            tt("dve", g2, ex, g1, ALU.mult, R, **W)
            tt("dve", gate_all[:, 2 * c:2 * c + 1], g1, pgrp, ALU.mult, R, joins=[b_gate])
            tt("dve", gate_all[:, 2 * c + 1:2 * c + 2], g2, pgrp, ALU.mult, R, joins=[b_gate])
            tt("dve", msel, oh1, oh2, ALU.add, R, **W)
            msb, b_msb = mselb[0]
            cp("dve", msb[:], msel, R, writes=[b_msb])
            def rank_and_scatter():
                pb, b_pb = pbs.next()
                mm(pb[:, 0:NE], sltb, msb[:], True, False, [b_cb16, b_msb], b_pb)
                mm(pb[:, 0:NE], onesb, mcum[:], False, True, [b_cb16, b_mcum], b_pb, newgrp=False)
                tt("dve", mcum[:], mcum[:], msb[:], ALU.add, [b_mcum, b_msb], writes=[b_mcum])
                tt("dve", rk, pb[:, 0:NE], cst[:, C_EC:C_EC + NE], ALU.add, [b_pb, b_cst], **W)
                for k, (oh, dk) in enumerate(((oh1, d1), (oh2, d2))):
                    tt("dve", tmp, oh, rk, ALU.mult, R, **W)
                    p.op("dve", lambda q, dk=dk: q.tensor_reduce(out=dk, in_=tmp, axis=AX.X, op=ALU.add), reads=R, **W)
                    cp("dve", dest_all[:, 2 * c + k:2 * c + k + 1], dk, R, joins=[b_dest])
                    p.dma_fn("pool", dsc[k], lambda q, k=k, hb_=hb_: q.indirect_dma_start(
                        out=buf_d, out_offset=bass.IndirectOffsetOnAxis(ap=dest_all[:, 2 * c + k:2 * c + k + 1], axis=0),
                        in_=hb_[:, :], in_offset=None),
                        reads=[b_hb, b_dest, b_bufz], joins=[b_bufd])

            pending[c] = rank_and_scatter
            yield

        def run_group(gens):
            gens = [g_ for g_ in gens if g_ is not None]
            while gens:
                for g_ in list(gens):
                    try:
                        next(g_)
                    except StopIteration:
                        gens.remove(g_)

        finals1 = []
        front_dma(0)
        front_pro(0)
        run_group([front(0)])
        if not stop:
            hz, b_hz = hnb[0]
            p.op("pool", lambda q: q.memset(hz[:], 0.0), writes=[b_hz])
            dzr = p.dsem()
            ZK = 16
            assert NSLOT % (P * ZK) == 0
            for zi in range(NSLOT // (P * ZK)):
                p.dma("sp", dzr, buf_d[zi * P * ZK:(zi + 1) * P * ZK, :].rearrange("(p k) d -> p k d", p=P),
                      hz[:].unsqueeze(1).to_broadcast([P, ZK, D]), reads=[b_hz], joins=[b_bufz])
        def delayed(g_, n):
            for _ in range(n):
                yield
            yield from g_

        for c in range(NCH):
            run_group([front(c + 1) if c + 1 < NCH else None, ssd(c), attn(c), trt(c - 1) if c > 0 else None])
        run_group([trt(NCH - 1)])
        if (NCH - 1) in pending:
            pending.pop(NCH - 1)()
        if stop:
            stats = p.emit(final_waits=finals1[-2:])
            return nc, stats

        p.barrier()
        aoff[0] = 0
        NWS = 3
        wgv = [big[:, i * 12288:i * 12288 + 4096].rearrange("p (j n) -> p j n", j=8) for i in range(NWS)]
        wuv = [big[:, i * 12288 + 4096:i * 12288 + 8192].rearrange("p (j n) -> p j n", j=8) for i in range(NWS)]
        wdv = [big[:, i * 12288 + 8192:i * 12288 + 12288].rearrange("p (j n) -> p j n", j=4) for i in range(NWS)]
        b_ws = [Buf("ws%d" % i) for i in range(NWS)]
        dws = [p.dsem() for _ in range(NWS)]
        xe = slots("xe", 4, [P, D], BF16)
        dxe = [p.dsem() for _ in range(4)]
        xeT = slots("xeT", 2, [P, 8, CAP], BF16)
        sg = slots("sg", 2, [P, 4, CAP], BF16)
        hTt = slots("hTt", 2, [P, 4, CAP], BF16)
        yo = slots("yo", 4, [P, D], F32)
        dyo = [p.dsem() for _ in range(4)]
        nx = 0
        ny = 0

        def load_w(e):
            i = e % NWS
            p.dma("pool", dws[i], wgv[i], wg_d[e].rearrange("(j p) n -> p j n", p=P), writes=[b_ws[i]])
            p.dma("pool", dws[i], wuv[i], wu_d[e].rearrange("(j p) n -> p j n", p=P), joins=[b_ws[i]])
            p.dma("pool", dws[i], wdv[i], wd_d[e].rearrange("(j p) n -> p j n", p=P), joins=[b_ws[i]])

        for e in range(min(2, NE)):
            load_w(e)
        def load_xe(e):
            for blk in range(NBLK):
                k_ = (e * NBLK + blk) % 4
                xet, b_xe = xe[k_]
                p.dma("sp", dxe[k_], xet[:], buf_d[e * CAP + blk * P:e * CAP + (blk + 1) * P, :], reads=[b_bufd], writes=[b_xe])

        assert NBLK == 2
        load_xe(0)
        for e in range(NE):
            if e + 2 < NE:
                load_w(e + 2)
            if e + 1 < NE:
                load_xe(e + 1)
            i = e % NWS
            xT_, b_xT_ = xeT[e % 2]
            for blk in range(NBLK):
                xet, b_xe = xe[(e * NBLK + blk) % 4]
                pt, b_pt = pts.next()
                for j in range(8):
                    tr(pt[:, j * P:(j + 1) * P], xet[:, j * P:(j + 1) * P], identb[:], [b_xe, b_identb], b_pt, j == 0)
                tt("dve", xT_[:, :, blk * P:(blk + 1) * P], pt[:].rearrange("p (j t) -> p j t", j=8),
                   gT[:, 16:24].unsqueeze(2).to_broadcast([P, 8, P]), ALU.mult, [b_pt, b_gT],
                   **(dict(writes=[b_xT_]) if blk == 0 else dict(joins=[b_xT_])))
            MPB = 512 // CAP
            sgt, b_sg = sg[e % 2]
            ht, b_ht = hTt[e % 2]
            gb = []
            for m in range(4):
                if m % MPB == 0:
                    gb.append(pbs.next())
                pb, b_pb = gb[-1]
                for j in range(8):
                    mm(pb[:, (m % MPB) * CAP:(m % MPB + 1) * CAP], wgv[i][:, j, m * P:(m + 1) * P], xT_[:, j, :], j == 0, j == 7,
                       [b_ws[i], b_xT_], b_pb, newgrp=(m % MPB == 0 and j == 0))
                if m % MPB == MPB - 1:
                    m0 = m - MPB + 1
                    act(sgt[:, m0:m + 1, :], pb[:, :].rearrange("p (a t) -> p a t", a=MPB), AF.Silu, [b_pb],
                        **(dict(writes=[b_sg]) if m0 == 0 else dict(joins=[b_sg])))
            for m in range(4):
                if m % MPB == 0:
                    gb.append(pbs.next())
                pb, b_pb = gb[-1]
                for j in range(8):
                    mm(pb[:, (m % MPB) * CAP:(m % MPB + 1) * CAP], wuv[i][:, j, m * P:(m + 1) * P], xT_[:, j, :], j == 0, j == 7,
                       [b_ws[i], b_xT_], b_pb, newgrp=(m % MPB == 0 and j == 0))
                if m % MPB == MPB - 1:
                    m0 = m - MPB + 1
                    tt("dve", ht[:, m0:m + 1, :], pb[:, :].rearrange("p (a t) -> p a t", a=MPB), sgt[:, m0:m + 1, :], ALU.mult,
                       [b_pb, b_sg], **(dict(writes=[b_ht]) if m0 == 0 else dict(joins=[b_ht])))
            for blk in range(NBLK):
                yot, b_yo = yo[ny % 4]
                for half in range(2):
                    pb, b_pb = pbs.next()
                    for m in range(4):
                        mm(pb[:, :], ht[:, m, blk * P:(blk + 1) * P], wdv[i][:, m, half * 512:(half + 1) * 512], m == 0, m == 3,
                           [b_ht, b_ws[i]], b_pb)
                    cp("act" if half == 0 else "dve", yot[:, half * 512:(half + 1) * 512], pb[:, :], [b_pb],
                       **(dict(writes=[b_yo]) if half == 0 else dict(joins=[b_yo])))
                p.dma("sp", dyo[ny % 4], ybuf_d[e * CAP + blk * P:e * CAP + (blk + 1) * P, :], yot[:], reads=[b_yo], joins=[b_ybufd])
                ny += 1

        p.barrier()
        aoff[0] = 0
        gfin, b_gfin = carve("gfin", [P, D], F32)
        dgf = p.dsem()
        p.dma("sp", dgf, gfin, gfin_d, writes=[b_gfin])
        junk, b_junk = carve("junk3", [P, D], BF16)
        r1 = slots("r1", 2, [P, D], F32)
        r2 = slots("r2", 2, [P, D], F32)
        h1r = slots("h1r", 2, [P, D], F32)
        ot_ = slots("ot", 2, [P, D], F32)
        st3 = slots("st3", 2, [P, 2], F32)
        dg = [[p.dsem() for _ in range(2)] for _ in range(2)]
        dhr = [p.dsem() for _ in range(2)]
        dot = [p.dsem() for _ in range(2)]
        finals = []
        for c in range(NCH):
            s = c % 2
            for k, (rr, b_rr) in enumerate((r1[s], r2[s])):
                p.dma_fn("pool", dg[s][k], lambda q, rr=rr, k=k, c=c: q.indirect_dma_start(
                    out=rr[:, :], out_offset=None, in_=ybuf_d[0:NSLOT, :],
                    in_offset=bass.IndirectOffsetOnAxis(ap=dest_all[:, 2 * c + k:2 * c + k + 1], axis=0)),
                    reads=[b_ybufd, b_dest], writes=[b_rr])
            hr, b_hr = h1r[s]
            if c == 0:
                p.dma("sp", dhr[s], hr[:], h1_d[c * P:(c + 1) * P, :], reads=[b_h1d], writes=[b_hr])
            if c + 1 < NCH:
                hrn, b_hrn = h1r[(c + 1) % 2]
                p.dma("sp", dhr[(c + 1) % 2], hrn[:], h1_d[(c + 1) * P:(c + 2) * P, :], reads=[b_h1d], writes=[b_hrn])
            stt(hr[:], r1[s][0][:], gate_all[:, 2 * c:2 * c + 1], hr[:], ALU.mult, ALU.add, [r1[s][1], b_gate, b_hr], writes=[b_hr])
            stt(hr[:], r2[s][0][:], gate_all[:, 2 * c + 1:2 * c + 2], hr[:], ALU.mult, ALU.add, [r2[s][1], b_gate, b_hr], writes=[b_hr])
            s3_, b_s3 = st3[s]
            act(junk[:], hr[:], AF.Square, [b_hr], writes=[b_junk, b_s3], accum_out=s3_[:, 0:1])
            rstd_from_ss(s3_[:, 0:1], s3_[:, 1:2], D, b_s3, b_s3)
            o_, b_o = ot_[s]
            stt(o_[:], hr[:], s3_[:, 1:2], gfin, ALU.mult, ALU.mult, [b_hr, b_s3, b_gfin], writes=[b_o])
            f = p.dma("sp", dot[s], out_d[c * P:(c + 1) * P, :], o_[:], reads=[b_o])
            finals.append(f)
        stats = p.emit(final_waits=finals[-2:])
    return nc, stats


def host_inputs(inp, S, NG, EPG, CAP, ncores):
    f = lambda a: np.ascontiguousarray(np.asarray(a, dtype=np.float32))
    w_in = f(inp["w_in"])[0]
    z, xbc, dt, q, k, v = np.split(w_in, [1024, 2560, 2576, 3600, 3728], axis=1)
    win = np.concatenate([z, xbc, q, k[:, 0:64], k[:, 0:64], k[:, 64:128], k[:, 64:128], v, dt], axis=1)
    assert win.shape[1] == NWIN
    cw = f(inp["conv_w"])[0]
    cwl = np.ascontiguousarray(cw.T.reshape(12, 128, 4).transpose(1, 0, 2).reshape(128, 48))
    sv = np.concatenate([np.broadcast_to(f(inp[n])[0][None, :], (128, 16)) for n in ("dt_bias", "a_log", "d_skip", "attn_sinks")], axis=1)
    tT = lambda v_: np.ascontiguousarray(f(v_).reshape(8, 128).T)
    gT = np.concatenate([tT(inp["ssd_norm"][0]), tT(inp["attn_norm"][0]), tT(inp["norm_ffn"][0]), tT(inp["norm_mix"][0])], axis=1)
    gfin = np.broadcast_to(f(inp["norm_final"])[None, :], (128, 1024))
    wr = np.concatenate([f(inp["w_router_group"])[0], f(inp["w_router_expert"])[0]], axis=1)
    NE = NG * EPG
    common = {
        "win": f(win), "wout": f(inp["w_out"])[0], "cw": f(cwl), "cb": f(inp["conv_b"])[0].reshape(1, 1536),
        "sv": f(sv), "gfin": f(gfin), "gT": f(gT), "wr": f(wr),
        "wg": f(inp["w_gate"])[0], "wu": f(inp["w_up"])[0], "wd": f(inp["w_down"])[0],
        "cst": make_consts(NE, CAP)[0], "cstb": make_consts(NE, CAP)[1],
    }
    x = f(inp["x"])
    maps = []
    for b in range(ncores):
        m = dict(common)
        m["x"] = np.ascontiguousarray(x[b])
        maps.append(m)
    return maps


_NC_CACHE = {}


def run_cfg(inp, S, NG, EPG, CAP, ncores, stop=0):
    key = (S, NG, EPG, CAP, stop)
    if key not in _NC_CACHE:
        _NC_CACHE[key] = build_nc(S, NG, EPG, CAP, stop)
    nc, stats = _NC_CACHE[key]
    maps = host_inputs(inp, S, NG, EPG, CAP, ncores)
    res = run_bass_kernel_spmd(nc, maps, core_ids=list(range(ncores)))
    return np.stack([np.asarray(r["out"], dtype=np.float32) for r in res.results], axis=0)


def kernel(**inputs):
    return run_cfg(inputs, 4096, 8, 8, 256, 8)
```
